# Optimizing a Trainium2 kernel written in Bass

```python
import math
import jax, jax.numpy as jnp
from jax import lax
import numpy as np

D_MODEL = 1024
BATCH = 4
SEQ = 4096
DEPTH = 2

HEAD_DIM = 64
N_ATTN_HEADS = 8
ATTN_WIDTH = N_ATTN_HEADS * HEAD_DIM
DILATED_BRANCHES = ((128, 1), (512, 4), (2048, 16))
N_SSD_HEADS = 4
SSD_HEAD_DIM = 64
SSD_WIDTH = N_SSD_HEADS * SSD_HEAD_DIM
SSD_GROUPS = 2
SSD_STATE = 128
CONV_WIDTH = 4
SSD_CHUNK = 128
CONV_CH = SSD_WIDTH + 2 * SSD_GROUPS * SSD_STATE
N_MEM_HEADS = 4
MEM_WIDTH = N_MEM_HEADS * HEAD_DIM
N_MEM = 256
MIX_WIDTH = ATTN_WIDTH + SSD_WIDTH + MEM_WIDTH
N_IN_COLS = 3 * ATTN_WIDTH + SSD_WIDTH + CONV_CH + N_SSD_HEADS + MEM_WIDTH
N_BUCKETS = 32
MAX_DISTANCE = 2048
N_EXPERTS = 16
N_EXPERT_GROUPS = 4
EXPERTS_PER_GROUP = N_EXPERTS // N_EXPERT_GROUPS
TOP_K = 2
D_EXPERT = 1024
ALPHA = (2 * DEPTH) ** 0.25
BETA = (8 * DEPTH) ** -0.25
LN_EPS = 1e-5
RMS_EPS = 1e-5

kernel_name = 'hybrid_dilated_ssd_memory_moe'


def _layer_norm(x, g, b):
    xf = x.astype(jnp.float32)
    mu = xf.mean(-1, keepdims=True)
    var = jnp.square(xf - mu).mean(-1, keepdims=True)
    return ((xf - mu) * lax.rsqrt(var + LN_EPS) * g + b).astype(x.dtype)


def _t5_bucket(dist):
    n = jnp.maximum(dist, 0)
    max_exact = N_BUCKETS // 2
    nf = jnp.maximum(n, max_exact).astype(jnp.float32)
    large = max_exact + (jnp.log(nf / max_exact) / math.log(MAX_DISTANCE / max_exact)
                         * (N_BUCKETS - max_exact)).astype(jnp.int32)
    large = jnp.minimum(large, N_BUCKETS - 1)
    return jnp.where(n < max_exact, n, large)


def _dilated_branch(q, k, v, rel_bias, window, dilation):
    b, s, h, e = q.shape
    steps_max = window // dilation
    blk = steps_max
    L = s // dilation
    nb = -(-L // blk)
    Lp = nb * blk

    def to_blocks(t):
        t = t.reshape(b, L, dilation, h, e).transpose(0, 2, 1, 3, 4)
        t = jnp.pad(t, ((0, 0), (0, 0), (0, Lp - L), (0, 0), (0, 0)))
        return t.reshape(b, dilation, nb, blk, h, e)

    def with_prev(t):
        prev = jnp.pad(t[:, :, :-1], ((0, 0), (0, 0), (1, 0), (0, 0), (0, 0), (0, 0)))
        return jnp.concatenate([prev, t], axis=3)

    qb = to_blocks(q)
    kk = with_prev(to_blocks(k))
    vv = with_prev(to_blocks(v))

    steps = jnp.arange(blk)[:, None] + blk - jnp.arange(2 * blk)[None, :]
    in_band = (steps >= 0) & (steps <= steps_max)
    has_prev = (jnp.arange(nb)[:, None, None] > 0) | (jnp.arange(2 * blk)[None, None, :] >= blk)
    valid = in_band[None] & has_prev
    bias = rel_bias[_t5_bucket(steps * dilation)].transpose(2, 0, 1).astype(jnp.float32)

    sc = jnp.einsum('brnqhe,brnkhe->brnhqk', qb, kk).astype(jnp.float32) + bias
    sc = jnp.where(valid[:, None], sc, -jnp.inf)
    m = sc.max(-1, keepdims=True)
    p = jnp.exp(sc - m)
    den = p.sum(-1, keepdims=True)
    o = jnp.einsum('brnhqk,brnkhe->brnqhe', p / den, vv.astype(jnp.float32))
    lse = (m + jnp.log(den))[..., 0]

    o = o.reshape(b, dilation, Lp, h, e)[:, :, :L].transpose(0, 2, 1, 3, 4).reshape(b, s, h, e)
    lse = lse.transpose(0, 1, 2, 4, 3).reshape(b, dilation, Lp, h)[:, :, :L]
    lse = lse.transpose(0, 2, 1, 3).reshape(b, s, h)
    return o, lse


def _dilated_attention(q, k, v, rel_bias):
    outs, lses = zip(*[_dilated_branch(q, k, v, rel_bias, w, d) for w, d in DILATED_BRANCHES])
    wts = jax.nn.softmax(jnp.stack(lses), axis=0)
    return jnp.einsum('ibsh,ibshe->bshe', wts, jnp.stack(outs))


def _causal_conv(u, w, bias):
    out = lax.conv_general_dilated(u, w[:, None, :], window_strides=(1,),
                                   padding=[(CONV_WIDTH - 1, 0)],
                                   dimension_numbers=('NWC', 'WIO', 'NWC'),
                                   feature_group_count=u.shape[-1])
    return out + bias


def _segsum(a):
    t = a.shape[-1]
    cs = jnp.cumsum(a, axis=-1)
    d = cs[..., :, None] - cs[..., None, :]
    return jnp.where(jnp.tril(jnp.ones((t, t), dtype=bool)), d, -jnp.inf)


def _ssd_chunked(xdt, adt, bm, cm):
    b, s, h, p = xdt.shape
    n = bm.shape[-1]
    c = s // SSD_CHUNK
    X = xdt.reshape(b, c, SSD_CHUNK, h, p)
    Bc = bm.reshape(b, c, SSD_CHUNK, h, n)
    Cc = cm.reshape(b, c, SSD_CHUNK, h, n)
    A = adt.reshape(b, c, SSD_CHUNK, h).transpose(0, 3, 1, 2)
    a_cs = jnp.cumsum(A, axis=-1)
    Lm = jnp.exp(_segsum(A))
    y_diag = jnp.einsum('bclhn,bcshn,bhcls,bcshp->bclhp', Cc, Bc, Lm, X)
    decay_states = jnp.exp(a_cs[..., -1:] - a_cs)
    states = jnp.einsum('bclhn,bhcl,bclhp->bchpn', Bc, decay_states, X)
    states = jnp.concatenate([jnp.zeros_like(states[:, :1]), states], axis=1)
    decay_chunk = jnp.exp(_segsum(jnp.pad(a_cs[..., -1], ((0, 0), (0, 0), (1, 0)))))
    states = jnp.einsum('bhzc,bchpn->bzhpn', decay_chunk, states)[:, :-1]
    y_off = jnp.einsum('bclhn,bchpn,bhcl->bclhp', Cc, states, jnp.exp(a_cs))
    return (y_diag + y_off).reshape(b, s, h, p)


def _ssd_mixer(z, xbc, dt_raw, conv_w, conv_b, dt_bias, a_log, d_skip, norm_w):
    b, s, _ = z.shape
    xbc = jax.nn.silu(_causal_conv(xbc, conv_w, conv_b)).astype(jnp.float32)
    xs, bm, cm = jnp.split(xbc, [SSD_WIDTH, SSD_WIDTH + SSD_GROUPS * SSD_STATE], axis=-1)
    xs = xs.reshape(b, s, N_SSD_HEADS, SSD_HEAD_DIM)
    rep = N_SSD_HEADS // SSD_GROUPS
    bm = jnp.repeat(bm.reshape(b, s, SSD_GROUPS, SSD_STATE), rep, axis=2)
    cm = jnp.repeat(cm.reshape(b, s, SSD_GROUPS, SSD_STATE), rep, axis=2)
    dt = jax.nn.softplus(dt_raw.astype(jnp.float32) + dt_bias.astype(jnp.float32))
    a = -jnp.exp(a_log.astype(jnp.float32))
    y = _ssd_chunked(xs * dt[..., None], a * dt, bm, cm) + d_skip.astype(jnp.float32)[:, None] * xs
    y = y.reshape(b, s, SSD_WIDTH) * jax.nn.silu(z.astype(jnp.float32))
    y = y * lax.rsqrt(jnp.mean(jnp.square(y), axis=-1, keepdims=True) + RMS_EPS) * norm_w
    return y.astype(z.dtype)


def _memory_attention(qm, mem, w_mem_kv, mem_bias):
    b, s, _ = qm.shape
    km, vm = jnp.split(mem @ w_mem_kv, 2, axis=-1)
    km = km.reshape(b, N_MEM, N_MEM_HEADS, HEAD_DIM)
    vm = vm.reshape(b, N_MEM, N_MEM_HEADS, HEAD_DIM)
    q = qm.reshape(b, s, N_MEM_HEADS, HEAD_DIM) * HEAD_DIM ** -0.5
    sc = jnp.einsum('bshe,bmhe->bhsm', q, km).astype(jnp.float32) + mem_bias.astype(jnp.float32)[None, :, None, :]
    p = jax.nn.softmax(sc, axis=-1)
    o = jnp.einsum('bhsm,bmhe->bshe', p, vm.astype(jnp.float32))
    return o.reshape(b, s, MEM_WIDTH).astype(qm.dtype)


def _moe(x, w_router, b_router, w_gate, w_up, w_down):
    logits = (x @ w_router).astype(jnp.float32) + b_router.astype(jnp.float32)
    scores = jax.nn.softmax(logits, axis=-1)
    grp = scores.reshape(*scores.shape[:-1], N_EXPERT_GROUPS, EXPERTS_PER_GROUP)
    grp_score = lax.top_k(grp, TOP_K)[0].sum(-1)
    best = jnp.argmax(grp_score, axis=-1)
    in_grp = (jnp.arange(N_EXPERTS) // EXPERTS_PER_GROUP) == best[..., None]
    top_w, top_i = lax.top_k(jnp.where(in_grp, scores, -jnp.inf), TOP_K)
    top_w = top_w / top_w.sum(-1, keepdims=True)
    gates = (jax.nn.one_hot(top_i, N_EXPERTS, dtype=jnp.float32) * top_w[..., None]).sum(-2)
    out = jnp.zeros_like(x)
    for e in range(N_EXPERTS):
        h = jax.nn.silu(x @ w_gate[e]) * (x @ w_up[e])
        out = out + gates[..., e:e + 1].astype(x.dtype) * (h @ w_down[e])
    return out


def setup_inputs(seed: int = 0) -> dict:
    key = jax.random.key(seed)
    ks = jax.random.split(key, 22)
    f32 = jnp.float32

    def nrm(k, shape, scale):
        return jax.random.normal(k, shape, f32) * scale

    x = nrm(ks[0], (BATCH, SEQ, D_MODEL), 1.0)
    mem = nrm(ks[1], (BATCH, N_MEM, D_MODEL), 1.0)
    col_scale = np.ones((N_IN_COLS,), np.float32)
    col_scale[2 * ATTN_WIDTH:3 * ATTN_WIDTH] = BETA
    w_in = nrm(ks[2], (DEPTH, D_MODEL, N_IN_COLS), D_MODEL ** -0.5) * jnp.asarray(col_scale)
    w_out = nrm(ks[3], (DEPTH, MIX_WIDTH, D_MODEL), BETA * MIX_WIDTH ** -0.5)
    rel_bias = nrm(ks[4], (N_BUCKETS, N_ATTN_HEADS), 0.1)
    conv_w = nrm(ks[5], (DEPTH, CONV_WIDTH, CONV_CH), CONV_WIDTH ** -0.5)
    conv_b = nrm(ks[6], (DEPTH, CONV_CH), 0.02)
    dt = jnp.exp(jax.random.uniform(ks[7], (DEPTH, N_SSD_HEADS), f32, math.log(1e-3), math.log(1e-1)))
    dt_bias = dt + jnp.log(-jnp.expm1(-dt))
    a_log = jnp.log(jax.random.uniform(ks[8], (DEPTH, N_SSD_HEADS), f32, 1.0, 16.0))
    d_skip = 1.0 + nrm(ks[9], (DEPTH, N_SSD_HEADS), 0.1)
    ssd_norm_w = 1.0 + nrm(ks[10], (DEPTH, SSD_WIDTH), 0.02)
    kv_scale = jnp.concatenate([jnp.ones((MEM_WIDTH,), f32), jnp.full((MEM_WIDTH,), BETA, f32)])
    w_mem_kv = nrm(ks[11], (DEPTH, D_MODEL, 2 * MEM_WIDTH), D_MODEL ** -0.5) * kv_scale
    mem_bias = nrm(ks[12], (DEPTH, N_MEM_HEADS, N_MEM), 0.1)
    ln1_g = 1.0 + nrm(ks[13], (DEPTH, D_MODEL), 0.02)
    ln1_b = nrm(ks[14], (DEPTH, D_MODEL), 0.02)
    ln2_g = 1.0 + nrm(ks[15], (DEPTH, D_MODEL), 0.02)
    ln2_b = nrm(ks[16], (DEPTH, D_MODEL), 0.02)
    w_router = nrm(ks[17], (D_MODEL, N_EXPERTS), D_MODEL ** -0.5)
    b_router = nrm(ks[18], (N_EXPERTS,), 0.01)
    w_gate = nrm(ks[19], (DEPTH, N_EXPERTS, D_MODEL, D_EXPERT), D_MODEL ** -0.5)
    w_up = nrm(ks[20], (DEPTH, N_EXPERTS, D_MODEL, D_EXPERT), BETA * D_MODEL ** -0.5)
    w_down = nrm(ks[21], (DEPTH, N_EXPERTS, D_EXPERT, D_MODEL), BETA * D_EXPERT ** -0.5)
    return {'x': x, 'mem': mem, 'w_in': w_in, 'w_out': w_out, 'rel_bias': rel_bias,
            'conv_w': conv_w, 'conv_b': conv_b, 'dt_bias': dt_bias, 'a_log': a_log,
            'd_skip': d_skip, 'ssd_norm_w': ssd_norm_w, 'w_mem_kv': w_mem_kv, 'mem_bias': mem_bias,
            'ln1_g': ln1_g, 'ln1_b': ln1_b, 'ln2_g': ln2_g, 'ln2_b': ln2_b,
            'w_router': w_router, 'b_router': b_router, 'w_gate': w_gate, 'w_up': w_up, 'w_down': w_down}


def reference(x, mem, w_in, w_out, rel_bias, conv_w, conv_b, dt_bias, a_log, d_skip, ssd_norm_w,
              w_mem_kv, mem_bias, ln1_g, ln1_b, ln2_g, ln2_b, w_router, b_router, w_gate, w_up, w_down):
    b, s, _ = x.shape
    splits = np.cumsum([ATTN_WIDTH, ATTN_WIDTH, ATTN_WIDTH, SSD_WIDTH, CONV_CH, N_SSD_HEADS]).tolist()
    for l in range(DEPTH):
        proj = x @ w_in[l]
        q, k, v, z, xbc, dt_raw, qm = jnp.split(proj, splits, axis=-1)
        heads = lambda t: t.reshape(b, s, N_ATTN_HEADS, HEAD_DIM)
        attn = _dilated_attention(heads(q) * HEAD_DIM ** -0.5, heads(k), heads(v), rel_bias)
        attn = attn.reshape(b, s, ATTN_WIDTH).astype(x.dtype)
        ssd = _ssd_mixer(z, xbc, dt_raw, conv_w[l], conv_b[l], dt_bias[l], a_log[l], d_skip[l], ssd_norm_w[l])
        memo = _memory_attention(qm, mem, w_mem_kv[l], mem_bias[l])
        y = jnp.concatenate([attn, ssd, memo], axis=-1) @ w_out[l]
        x = _layer_norm(ALPHA * x + y, ln1_g[l], ln1_b[l])
        y = _moe(x, w_router, b_router, w_gate[l], w_up[l], w_down[l])
        x = _layer_norm(ALPHA * x + y, ln2_g[l], ln2_b[l])
    return x
```

```python
import math
import numpy as np
import concourse.bass as bass
import concourse.mybir as mybir
from concourse.bass_utils import run_bass_kernel_spmd

F32 = mybir.dt.float32
BF16 = mybir.dt.bfloat16
ALU = mybir.AluOpType
AF = mybir.ActivationFunctionType

D = 1024
TOK = 2048
NT = 16
DEPTH = 2
N_IN = 2820
ALPHA = (2 * DEPTH) ** 0.25
LN_EPS = 1e-5
RMS_EPS = 1e-5
NE = 16
BRANCH_D = (1, 4, 16)
LG = 384
NEG = -30000.0
SAME_ENG_SYNC = True


_ALLT = []


class T:
    __slots__ = ("name", "w", "r", "sem", "cnt")

    def __init__(self, name=""):
        self.name = name
        self.w = None
        self.r = {}
        self.sem = None
        self.cnt = 0
        _ALLT.append(self)


class Sched:
    ENGS = ("pe", "act", "dve", "pool", "sp")

    def __init__(self, nc):
        self.nc = nc
        self.ops = []

    def op(self, eng, fn, reads=(), writes=(), dma=False, inc=16):
        deps = set()
        for t in reads:
            if t.w is not None:
                deps.add(t.w)
        for t in writes:
            if t.w is not None:
                deps.add(t.w)
            deps.update(t.r.values())
        idx = len(self.ops)
        dt = None
        if dma:
            assert len(writes) == 1
            dt = writes[0]
        best = {}
        red = set()
        for d in deps:
            Dp = self.ops[d]
            if Dp["dma"]:
                red.add(d)
            elif best.get(Dp["eng"], -1) < d:
                best[Dp["eng"]] = d
        red.update(best.values())
        self.ops.append(dict(eng=eng, fn=fn, deps=red, dma=dma, dt=dt, sig=False, cnt=0, dinc=inc))
        for t in reads:
            if dma:
                t.r[("dma", idx)] = idx
            else:
                t.r[eng] = idx
        for t in writes:
            t.w = idx
            t.r = {}
        return idx

    def emit(self, final_tiles=()):
        nc = self.nc
        ops = self.ops
        fdeps = set()
        for t in final_tiles:
            if t.w is not None:
                fdeps.add(t.w)
        ops.append(dict(eng="sp", fn=None, deps=fdeps, dma=False, dt=None, sig=False, cnt=0, dinc=16))
        for i, o in enumerate(ops):
            for d in o["deps"]:
                Dp = ops[d]
                if Dp["dma"]:
                    Dp["sig"] = True
                elif Dp["eng"] != o["eng"] or o["dma"]:
                    Dp["sig"] = True
                elif o["eng"] != "pe" and SAME_ENG_SYNC:
                    Dp["sig"] = True
        for o in ops:
            if o["dma"]:
                o["sig"] = True
        semreg = {}
        esem = {e: nc.alloc_semaphore(name="prog_" + e) for e in self.ENGS}
        ecnt = {e: 0 for e in self.ENGS}
        for o in ops:
            if not o["sig"]:
                continue
            if o["dma"]:
                t = o["dt"]
                if t.sem is None:
                    if t.name in semreg:
                        prev = semreg[t.name]
                        t.sem, t.cnt = prev.sem, prev.cnt
                    else:
                        t.sem = nc.alloc_semaphore(name="dma_" + t.name)
                    semreg[t.name] = t
                t.cnt += o["dinc"]
                o["sem"] = t.sem
                o["cnt"] = t.cnt
                o["inc"] = o["dinc"]
            else:
                ecnt[o["eng"]] += 1
                o["sem"] = esem[o["eng"]]
                o["cnt"] = ecnt[o["eng"]]
                o["inc"] = 1
        per = {e: [] for e in self.ENGS}
        for i, o in enumerate(ops):
            per[o["eng"]].append(i)
        nwaits = [0]

        def run(engname, engine):
            waited = {}
            for i in per[engname]:
                o = ops[i]
                need = {}
                for d in o["deps"]:
                    Dp = ops[d]
                    if not Dp["sig"]:
                        continue
                    if (not Dp["dma"]) and Dp["eng"] == engname and not o["dma"]:
                        if engname == "pe" or not SAME_ENG_SYNC:
                            continue
                    s = Dp["sem"]
                    key = id(s)
                    if need.get(key, (None, 0))[1] < Dp["cnt"]:
                        need[key] = (s, Dp["cnt"])
                for key, (s, c) in need.items():
                    if waited.get(key, 0) >= c:
                        continue
                    engine.wait_ge(s, c)
                    nwaits[0] += 1
                    waited[key] = c
                if o["fn"] is None:
                    continue
                ins = o["fn"](engine)
                if o["sig"]:
                    ins.then_inc(o["sem"], o["inc"])

        with nc.Block() as block:
            @block.sync
            def _(e):
                run("sp", e)

            @block.tensor
            def _(e):
                run("pe", e)

            @block.scalar
            def _(e):
                run("act", e)

            @block.vector
            def _(e):
                run("dve", e)

            @block.gpsimd
            def _(e):
                run("pool", e)
        self.stats = dict(nops=len(ops), nwaits=nwaits[0], counts=dict(ecnt))


def _t5_bucket_np(dist):
    n = np.maximum(dist, 0)
    max_exact = 16
    nf = np.maximum(n, max_exact).astype(np.float32)
    val = np.log(nf / np.float32(max_exact)) / np.float32(math.log(2048 / max_exact)) * np.float32(32 - max_exact)
    large = max_exact + val.astype(np.float32).astype(np.int32)
    large = np.minimum(large, 31)
    return np.where(n < max_exact, n, large)


def _onehot_table():
    oh = np.zeros((32, 3 * LG), np.float32)
    steps = np.arange(0, 129)
    for bi, d in enumerate(BRANCH_D):
        b = _t5_bucket_np(steps * d)
        for s_, u in zip(steps, b):
            oh[int(u), bi * LG + int(s_) + 127] = 1.0
    return oh


class Prog:
    def __init__(self, nlayers, debug=False, stop=None):
        self.nl = nlayers
        self.debug = debug
        self.stop = stop
        nc = bass.Bass("TRN2", target_bir_lowering=False)
        del _ALLT[:]
        self.nc = nc
        self.S = Sched(nc)
        L = nlayers
        di = lambda name, shape: nc.dram_tensor(name, shape, F32, kind="ExternalInput").ap()
        self.x_own = di("x_own", [TOK, D])
        self.x_pre = di("x_pre", [TOK, D])
        self.flag_d = di("flag", [128, 1])
        self.mem_d = di("mem", [256, D])
        self.w_in = di("w_in", [L, D, N_IN])
        self.w_out = di("w_out", [L, D, D])
        self.rel_bias = di("rel_bias", [32, 8])
        self.oh_d = di("oh", [32, 3 * LG])
        self.conv_w = di("conv_w", [L, 4, 768])
        self.conv_b = di("conv_b", [L, 768])
        self.dt_bias = di("dt_bias", [L, 4])
        self.a_log = di("a_log", [L, 4])
        self.d_skip = di("d_skip", [L, 4])
        self.ssd_nw = di("ssd_norm_w", [L, 256])
        self.w_mem_kv = di("w_mem_kv", [L, D, 512])
        self.mem_bias = di("mem_bias", [L, 4, 256])
        self.ln1_g = di("ln1_g", [L, D])
        self.ln1_b = di("ln1_b", [L, D])
        self.ln2_g = di("ln2_g", [L, D])
        self.ln2_b = di("ln2_b", [L, D])
        self.w_router = di("w_router", [D, NE])
        self.b_router = di("b_router", [1, NE])
        self.w_gate = di("w_gate", [L, NE, D, D])
        self.w_up = di("w_up", [L, NE, D, D])
        self.w_down = di("w_down", [L, NE, D, D])
        self.out_d = nc.dram_tensor("x_out", [TOK, D], F32, kind="ExternalOutput").ap()
        self.escr = nc.dram_tensor("escr", [24, 128 * (LG + 1)], F32, kind="Internal").ap()
        self.xmid = nc.dram_tensor("xmid", [TOK, D], F32, kind="Internal").ap()
        self.xg = nc.dram_tensor("xg", [4 * 2 * 512, D], F32, kind="Internal").ap()
        if debug:
            self.dbg_mix = nc.dram_tensor("dbg_mix", [128, 8 * TOK], BF16, kind="ExternalOutput").ap()
            self.dbg_x1 = nc.dram_tensor("dbg_x1", [TOK, D], F32, kind="ExternalOutput").ap()
        self.arena = nc.alloc_sbuf_tensor("arena", [128, 51968], F32)
        self.apos = 0
        self.pbanks = [(nc.alloc_psum_tensor("pb%d" % i, [128, 512], F32)[:, :], T("pb%d" % i)) for i in range(6)]
        self.pi = 0
        self.pbf = (nc.alloc_psum_tensor("pbf", [128, 1024], BF16)[:, :], T("pbf"))
        self.plong = (nc.alloc_psum_tensor("plong", [128, 512], F32)[:, :], T("plong"))
        self.pb8 = self.pbanks + [self.plong, (self.pbf[0].bitcast(F32), self.pbf[1])]
        self.pi8 = 0

    def region(self, words):
        a = self.apos
        self.apos += words
        assert self.apos <= 51968, self.apos
        return a

    def f32(self, off, n):
        return self.arena[:, off:off + n]

    def bf(self, off_words, n_bf):
        assert n_bf % 2 == 0
        return self.arena[:, off_words:off_words + n_bf // 2].bitcast(BF16)

    def ps(self):
        p = self.pbanks[self.pi % 6]
        self.pi += 1
        return p

    def ps8(self):
        p = self.pb8[self.pi8 % 8]
        self.pi8 += 1
        return p

    def MM(self, out, lhsT, rhs, start, stop, R, W):
        self.S.op("pe", lambda e: e.matmul(out, lhsT=lhsT, rhs=rhs, start=start, stop=stop), R, W)

    def TR(self, out, in_, ident, R, W):
        self.S.op("pe", lambda e: e.transpose(out=out, in_=in_, identity=ident), R, W)

    def ACT(self, out, in_, func, R, W, **kw):
        self.S.op("act", lambda e: e.activation(out=out, in_=in_, func=func, **kw), R, W)

    def TT(self, eng, out, in0, in1, op, R, W):
        self.S.op(eng, lambda e: e.tensor_tensor(out=out, in0=in0, in1=in1, op=op), R, W)

    def TS(self, eng, out, in0, s1, s2, op0, op1, R, W):
        if op1 is None:
            self.S.op(eng, lambda e: e.tensor_scalar(out=out, in0=in0, scalar1=s1, scalar2=None, op0=op0), R, W)
        else:
            self.S.op(eng, lambda e: e.tensor_scalar(out=out, in0=in0, scalar1=s1, scalar2=s2, op0=op0, op1=op1), R, W)

    def STT(self, eng, out, in0, scalar, in1, op0, op1, R, W):
        self.S.op(eng, lambda e: e.scalar_tensor_tensor(out=out, in0=in0, scalar=scalar, in1=in1, op0=op0, op1=op1), R, W)

    def CP(self, eng, out, in_, R, W):
        if eng == "act":
            self.ACT(out, in_, AF.Copy, R, W)
        else:
            self.S.op(eng, lambda e: e.tensor_copy(out=out, in_=in_), R, W)

    def DMA(self, eng, out, in_, R, W, slow=False):
        if slow:
            self.S.op(eng, lambda e: e.dma_start(out=out, in_=in_, allow_slow_non_contiguous=True), R, W, dma=True)
        else:
            self.S.op(eng, lambda e: e.dma_start(out=out, in_=in_), R, W, dma=True)

    def RECIP(self, out, in_, R, W):
        self.S.op("dve", lambda e: e.reciprocal(out=out, in_=in_), R, W)

    def REDUCE(self, out, in_, op, R, W):
        self.S.op("dve", lambda e: e.tensor_reduce(out=out, in_=in_, axis=mybir.AxisListType.X, op=op), R, W)

    def BNS(self, out, in_, R, W):
        self.S.op("dve", lambda e: e.bn_stats(out=out, in_=in_), R, W)

    def BNA(self, out, in_, R, W):
        self.S.op("dve", lambda e: e.bn_aggr(out=out, in_=in_), R, W)

    def MEMSET(self, eng, ap, val, R, W):
        self.S.op(eng, lambda e: e.memset(ap, val), R, W)

    def ASEL(self, out, in_, pattern, cmp, fill, cm, R, W):
        self.S.op("pool", lambda e: e.affine_select(out=out, in_=in_, pattern=pattern, compare_op=cmp,
                                                     fill=fill, base=0, channel_multiplier=cm), R, W)

    def transfer(self, old, new, scratch, Tscr):
        self.S.op("pool", lambda e: e.memset(scratch, 0.0), [], [Tscr] + list(old) + list(new))

    def setup(self):
        P = self
        c = {}
        self.c = c
        off = P.region(128); c["ident_f"] = P.f32(off, 128)
        off = P.region(128); c["tri"] = P.f32(off, 128)
        off = P.region(128); c["su"] = P.f32(off, 128)
        off = P.region(128); c["neg"] = P.f32(off, 128)
        off = P.region(128); c["ones_f"] = P.f32(off, 128)
        off = P.region(64); c["ident_b"] = P.bf(off, 128)
        off = P.region(64); c["ones_b"] = P.bf(off, 128)
        off = P.region(32); c["flagm"] = P.bf(off, 64)
        off = P.region(2); c["flag"] = P.f32(off, 1); c["scr"] = P.f32(off + 1, 1)
        off = P.region(16); c["bar"] = P.f32(off, 16)
        off = P.region(3 * 8 * 128); c["etab"] = P.bf(off, 3 * 8 * 256).rearrange("p (b h c) -> p b h c", b=3, h=8)
        Tc = T("consts")
        self.Tc = Tc
        self.Tscr = T("scr")
        self.Tflag = T("flag")
        self.Tetab = T("etab")
        ones_f = c["ones_f"]
        P.MEMSET("pool", ones_f, 1.0, [], [Tc])
        P.MEMSET("pool", c["ones_b"], 1.0, [], [Tc])
        P.MEMSET("pool", c["scr"], 0.0, [], [self.Tscr])
        P.ASEL(c["ident_f"], ones_f, [[-1, 128]], ALU.is_equal, 0.0, 1, [Tc], [Tc])
        P.ASEL(c["tri"], ones_f, [[1, 128]], ALU.is_ge, 0.0, -1, [Tc], [Tc])
        P.ASEL(c["su"], ones_f, [[-1, 128]], ALU.is_gt, 0.0, 1, [Tc], [Tc])
        P.MEMSET("pool", c["neg"], NEG, [], [Tc])
        P.ASEL(c["neg"], c["neg"], [[-1, 128]], ALU.is_gt, 0.0, 1, [Tc], [Tc])
        P.CP("pool", c["ident_b"], c["ident_f"], [Tc], [Tc])
        P.DMA("sp", c["flag"], self.flag_d, [], [self.Tflag])
        P.TS("dve", c["flagm"], c["ones_b"][:, 0:64], c["flag"][:, 0:1], None, ALU.mult, None, [Tc, self.Tflag], [Tc])

    def build_etab(self, tmp_off):
        P = self
        c = self.c
        o = tmp_off
        rb = P.f32(o, 8)[0:32, :]; o += 8
        lh = P.f32(o, 128)[0:32, :]; o += 128
        oh = P.f32(o, 3 * LG)[0:32, :]; o += 3 * LG
        gsb = [P.f32(o + i * LG, LG) for i in range(2)]; o += 2 * LG
        Trb, Tlh, Toh = T("rb"), T("lh"), T("oh")
        Tg = [T("gsb0"), T("gsb1")]
        Tes2 = [T("escr0"), T("escr1")]
        P.DMA("sp", rb, self.rel_bias, [], [Trb])
        P.DMA("sp", oh, self.oh_d, [], [Toh])
        P.ACT(rb, rb, AF.Exp, [Trb], [Trb])
        k = 0
        for h in range(8):
            P.TS("dve", lh, c["ones_f"][0:32, :], rb[:, h:h + 1], None, ALU.mult, None, [self.Tc, Trb], [Tlh])
            for b in range(3):
                ps, Tps = P.ps()
                P.MM(ps[:, 0:LG], lh, oh[:, b * LG:(b + 1) * LG], True, True, [Tlh, Toh], [Tps])
                g, Tgk = gsb[k % 2], Tg[k % 2]
                P.CP("act", g, ps[:, 0:LG], [Tps], [Tgk])
                idx = h * 3 + b
                dst = self.escr[idx].rearrange("(p c) -> p c", c=LG + 1)[:, 0:LG]
                Tes = Tes2[k % 2]
                P.DMA("sp", dst, g, [Tgk], [Tes])
                src = bass.AP(tensor=self.escr.tensor, offset=idx * 128 * (LG + 1) + 127,
                              ap=[[LG, 128], [128, 2], [1, 128]])
                P.DMA("pool", c["etab"][:, b, h, :].rearrange("p (a q) -> p a q", a=2), src, [Tes], [self.Tetab])
                k += 1
                yield None
        return [Trb, Tlh, Toh] + Tg

    def load_xT(self, x_fn, xT, TxT, stage, Tstage):
        P = self
        c = self.c
        for tt in range(NT):
            st, Tst = stage[tt % 2], Tstage[tt % 2]
            x_ap, x_R = x_fn(tt)
            P.DMA("sp", st, x_ap, x_R, [Tst])
            for half in range(2):
                ps, Tps = P.ps()
                for q in range(4):
                    kc = half * 4 + q
                    P.TR(ps[:, q * 128:(q + 1) * 128], st[:, kc * 128:(kc + 1) * 128], c["ident_f"], [Tst, self.Tc], [Tps])
                P.CP("act" if half == 0 else "dve", xT[:, half * 4:(half + 1) * 4, tt * 128:(tt + 1) * 128],
                     ps.rearrange("p (q t) -> p q t", q=4), [Tps], [TxT])

    def wload(self, dst, src2d, R, W):
        self.DMA("pool", dst, src2d.rearrange("(k p) n -> p k n", p=128), R, W)

    def layer(self, l, x_own, x_pre, out_d):
        P = self
        c = self.c
        base = self.apos
        R0 = P.region(8192)
        R1 = P.region(8192)
        R2 = P.region(8192)
        R3 = P.region(16384)
        R4 = P.region(4096)
        R5 = P.region(2048 + 512 + 64)
        xT = P.bf(R0, 8 * TOK).rearrange("p (k t) -> p k t", k=8)
        xTp = P.bf(R1, 8 * TOK).rearrange("p (k t) -> p k t", k=8)
        mixT = P.bf(R2, 8 * TOK).rearrange("p (k t) -> p k t", k=8)
        stage = [P.f32(R5, 1024), P.f32(R5 + 1024, 1024)]
        Tstage = [T("stage0"), T("stage1")]
        small = R5 + 2048
        TxT, TxTp, Tmix = T("xT"), T("xTp"), T("mixT")
        W_in = self.w_in[l]

        wq = [P.bf(R4 + i * 1536, 3 * 8 * 128).rearrange("p (s k n) -> p s k n", s=3, k=8) for i in range(2)]
        Twq = [T("wq0"), T("wq1")]
        for s_ in range(3):
            P.wload(wq[0][:, s_], W_in[:, s_ * 512:s_ * 512 + 128], [], [Twq[0]])
        P.load_xT(x_own, xT, TxT, stage, Tstage)
        P.load_xT(x_pre, xTp, TxTp, stage, Tstage)
        etab_state = {"gen": self.build_etab(R2) if l == 0 else None, "tmp": []}

        def etab_step():
            g_ = etab_state["gen"]
            if g_ is None:
                return
            try:
                next(g_)
            except StopIteration as e_:
                etab_state["tmp"] = e_.value
                etab_state["gen"] = None

        if self.stop == "etab":
            Td = T("dbgmix")
            P.DMA("sp", self.dbg_mix[:, 0:6144], c["etab"].rearrange("p b h c -> p (b h c)"), [self.Tetab], [Td])
            self.final.append(Td)
            return
        if self.stop == "xT":
            Td = T("dbgmix")
            P.DMA("sp", self.dbg_mix, xT.rearrange("p k t -> p (k t)"), [TxT, TxTp], [Td])
            self.final.append(Td)
            return
        o = R3
        QT = []; KT = []; Vo = []; KpT = []; Vp = []
        for bi, d in enumerate(BRANCH_D):
            QT.append(P.bf(o, TOK)); o += TOK // 2
        for bi, d in enumerate(BRANCH_D):
            KT.append(P.bf(o, TOK)); o += TOK // 2
        npre = {1: 1, 4: 4, 16: 16}
        for bi, d in enumerate(BRANCH_D):
            KpT.append(P.bf(o, npre[d] * 128)); o += npre[d] * 64
        for bi, d in enumerate(BRANCH_D):
            Vo.append(P.bf(o, 16 * 128).rearrange("p (j c) -> p j c", j=16)); o += 16 * 64
        for bi, d in enumerate(BRANCH_D):
            Vp.append(P.bf(o, npre[d] * 128).rearrange("p (j c) -> p j c", j=npre[d])); o += npre[d] * 64
        accND = P.f32(o, 2 * TOK).rearrange("p (a t) -> p a t", a=2); o += 2 * TOK
        assert o <= R3 + 16384, o - R3
        Psb = [P.bf(R5, 512), P.bf(R5 + 1024, 512), P.bf(R5 + 256, 512), P.bf(R5 + 1280, 512)]
        TPsb = [T("Psb%d" % i) for i in range(4)]
        P.transfer(Tstage, TPsb, c["scr"], self.Tscr)
        TQ = [T("QT%d" % i) for i in range(3)]
        TK = [T("KT%d" % i) for i in range(3)]
        TKp = [T("KpT%d" % i) for i in range(3)]
        TV = [T("V%d" % i) for i in range(3)]
        TVp = [T("Vp%d" % i) for i in range(3)]
        Tacc = T("accND")
        tmpND = P.f32(R2 + 4096, 2 * TOK).rearrange("p (a t) -> p a t", a=2)
        Ttmp = T("tmpND")

        def evac_reorder(eng, dst_flat, ps, d, tg, R, W, scale=None):
            if d == 1:
                o_ap = dst_flat[:, tg * 512:(tg + 1) * 512]
                i_ap = ps
            else:
                o_ap = dst_flat.rearrange("p (r m) -> p r m", r=d)[:, :, tg * (512 // d):(tg + 1) * (512 // d)]
                i_ap = ps.rearrange("p (m r) -> p r m", r=d)
            if eng == "act":
                if scale is None:
                    P.ACT(o_ap, i_ap, AF.Copy, R, W)
                else:
                    P.ACT(o_ap, i_ap, AF.Copy, R, W, scale=scale)
            else:
                if scale is None:
                    P.CP("dve", o_ap, i_ap, R, W)
                else:
                    P.TS("dve", o_ap, i_ap, scale, None, ALU.mult, None, R, W)

        for hp in range(4):
            wb, Twb = wq[hp % 2], Twq[hp % 2]
            for s_ in range(3):
                col = s_ * 512 + hp * 128
                if hp > 0:
                    P.wload(wb[:, s_], W_in[:, col:col + 128], [], [Twb])
            for which in range(3):
                src_xT, Tsrc = (xT, TxT) if which < 2 else (xTp, TxTp)
                wsel = wb[:, 0] if which == 0 else wb[:, 1]
                for tg in range(4):
                    ps, Tps = P.ps()
                    for kc in range(8):
                        P.MM(ps, wsel[:, kc, :], src_xT[:, kc, tg * 512:(tg + 1) * 512], kc == 0, kc == 7, [Twb, Tsrc], [Tps])
                    for bi, d in enumerate(BRANCH_D):
                        eng = "act"
                        if which == 0:
                            evac_reorder(eng, QT[bi], ps, d, tg, [Tps], [TQ[bi]], scale=0.125)
                        elif which == 1:
                            evac_reorder(eng, KT[bi], ps, d, tg, [Tps], [TK[bi]])
                        else:
                            if d == 16:
                                evac_reorder(eng, KpT[bi], ps, d, tg, [Tps], [TKp[bi]])
                            elif d == 4 and tg == 3:
                                P.CP(eng, KpT[bi].rearrange("p (r m) -> p r m", r=4), ps.rearrange("p (m r) -> p r m", r=4), [Tps], [TKp[bi]])
                            elif d == 1 and tg == 3:
                                P.CP(eng, KpT[bi], ps[:, 384:512], [Tps], [TKp[bi]])
                    if hp == 0:
                        etab_step()
            if self.stop == "qk":
                return
            for bi, d in enumerate(BRANCH_D):
                nblk = 16 // d
                for own in (True, False):
                    src_xT, Tsrc = (xT, TxT) if own else (xTp, TxTp)
                    if own:
                        tiles = [(j, (j // nblk), (j % nblk)) for j in range(16)]
                    else:
                        tiles = [(r, r, nblk - 1) for r in range(d)]
                    dstV, TdV = (Vo[bi], TV[bi]) if own else (Vp[bi], TVp[bi])
                    for g0 in range(0, len(tiles), 4):
                        grp = tiles[g0:g0 + 4]
                        ps, Tps = P.ps()
                        for gi, (j, r, n) in enumerate(grp):
                            start = n * 128 * d + r
                            for kc in range(8):
                                P.MM(ps[:, gi * 128:(gi + 1) * 128], src_xT[:, kc, start:start + 127 * d + 1:d], wb[:, 2, kc, :],
                                     kc == 0, kc == 7, [Twb, Tsrc], [Tps])
                        ng = len(grp)
                        if own:
                            P.CP("act" if (g0 // 4) % 2 == 0 else "dve", dstV[:, g0:g0 + ng, :],
                                 ps[:, 0:ng * 128].rearrange("p (j c) -> p j c", j=ng), [Tps], [TdV])
                        else:
                            P.TS("dve", dstV[:, g0:g0 + ng, :], ps[:, 0:ng * 128].rearrange("p (j c) -> p j c", j=ng),
                                 c["flag"][:, 0:1], None, ALU.mult, None, [Tps, self.Tflag], [TdV])
                        if hp == 0:
                            etab_step()
            if self.stop == "proj":
                return
            if hp == 0:
                while etab_state["gen"] is not None:
                    etab_step()
                if etab_state["tmp"]:
                    P.transfer(etab_state["tmp"], [Tmix, Ttmp], c["scr"], self.Tscr)
                    etab_state["tmp"] = []
            units = []
            for bi, d in enumerate(BRANCH_D):
                if self.stop == "b0" and bi > 0:
                    break
                for j in range(16):
                    units.append((bi, d, j))
            ust = {}

            def stage_scores(i):
                bi, d, j = units[i]
                nblk = 16 // d
                r, n = j // nblk, j % nblk
                if n > 0:
                    pK = KT[bi][:, (j - 1) * 128:j * 128]; TpK = TK[bi]
                    pV = Vo[bi][:, j - 1, :]; TpV = TV[bi]
                    pden = c["ones_b"][:, 0:64]
                else:
                    pK = KpT[bi][:, r * 128:(r + 1) * 128]; TpK = TKp[bi]
                    pV = Vp[bi][:, r, :]; TpV = TVp[bi]
                    pden = c["flagm"][:, 0:64]
                pss = [P.ps8(), P.ps8()]
                for hh in range(2):
                    ps, Tps = pss[hh]
                    rows = slice(hh * 64, hh * 64 + 64)
                    qv = QT[bi][rows, j * 128:(j + 1) * 128]
                    P.MM(ps[:, 128:256], pK[rows], qv, True, True, [TpK, TQ[bi]], [Tps])
                    P.MM(ps[:, 0:128], KT[bi][rows, j * 128:(j + 1) * 128], qv, True, True, [TK[bi], TQ[bi]], [Tps])
                pb, Tpb = Psb[i % 3], TPsb[i % 3]
                for hh in range(2):
                    ps, Tps = pss[hh]
                    P.ACT(pb[:, hh * 256:(hh + 1) * 256], ps[:, 0:256], AF.Exp, [Tps], [Tpb])
                P.TT("dve", pb, pb, c["etab"][:, bi, 2 * hp:2 * hp + 2, :].rearrange("p h c -> p (h c)"), ALU.mult, [Tpb, self.Tetab], [Tpb])
                ust[i] = (pb, Tpb, pV, TpV, pden, r, n)

            def stage_pv(i):
                bi, d, j = units[i]
                pb, Tpb, pV, TpV, pden, r, n = ust.pop(i)
                ps2, Tps2 = P.ps8()
                for hh in range(2):
                    rows = slice(hh * 64, hh * 64 + 64)
                    p_own = pb[:, hh * 256:hh * 256 + 128]
                    p_prev = pb[:, hh * 256 + 128:hh * 256 + 256]
                    P.MM(ps2[rows, 0:128], pV[:, hh * 64:(hh + 1) * 64], p_prev, True, False, [TpV, Tpb], [Tps2])
                    P.MM(ps2[rows, 0:128], Vo[bi][:, j, hh * 64:(hh + 1) * 64], p_own, False, True, [TV[bi], Tpb], [Tps2])
                    P.MM(ps2[rows, 128:256], pden, p_prev, True, False, [self.Tc, Tpb], [Tps2])
                    P.MM(ps2[rows, 128:256], c["ones_b"][:, 0:64], p_own, False, True, [self.Tc, Tpb], [Tps2])
                start = n * 128 * d + r
                p_ap = ps2[:, 0:256].rearrange("p (a q) -> p a q", a=2)
                if bi == 0:
                    P.CP("act", accND[:, :, start:start + 128], p_ap, [Tps2], [Tacc])
                else:
                    P.CP("act", tmpND[:, :, start:start + 127 * d + 1:d], p_ap, [Tps2], [Ttmp])
                    if j == 15:
                        P.TT("dve", accND.rearrange("p a t -> p (a t)"), accND.rearrange("p a t -> p (a t)"),
                             tmpND.rearrange("p a t -> p (a t)"), ALU.add, [Tacc, Ttmp], [Tacc])

            for i in range(len(units) + 2):
                if i < len(units):
                    stage_scores(i)
                if i >= 2:
                    stage_pv(i - 2)
            P.RECIP(accND[:, 1, :], accND[:, 1, :], [Tacc], [Tacc])
            P.TT("dve", mixT[:, hp, :], accND[:, 0, :], accND[:, 1, :], ALU.mult, [Tacc], [Tmix])

        if self.stop in ("attn", "b0"):
            Td = T("dbgmix")
            P.DMA("sp", self.dbg_mix, mixT.rearrange("p k t -> p (k t)"), [Tmix], [Td])
            self.final.append(Td)
            return
        P.transfer([Ttmp], [Tmix], c["scr"], self.Tscr)
        P.transfer(TPsb, Tstage, c["scr"], self.Tscr)
        old = TQ + TK + TKp + TV + TVp + [Tacc]
        o = R3
        memT = P.bf(o, 8 * 256).rearrange("p (k m) -> p k m", k=8); o += 1024
        kmT = P.bf(o, 2 * 256).rearrange("p (k m) -> p k m", k=2); o += 256
        vm = P.bf(o, 2 * 256).rearrange("p (k m) -> p k m", k=2); o += 256
        qmT = P.bf(o, 2 * TOK).rearrange("p (k t) -> p k t", k=2); o += TOK
        Pm = [[P.bf(o + (i * 2 + mc) * 256, 512) for mc in range(2)] for i in range(2)]; o += 1024
        rec = P.f32(o, 512); o += 512
        mb = P.f32(o, 8).rearrange("p (h m) -> p h m", h=4); o += 8
        TmemT, Tkm, Tvm, Tqm, Trec, Tmb = T("memT"), T("kmT"), T("vm"), T("qmT"), T("rec"), T("mb")
        TPm = [T("Pm0"), T("Pm1")]
        P.transfer(old, [TmemT, Tkm, Tvm, Tqm, Trec, Tmb] + TPm, c["scr"], self.Tscr)
        wkv = P.bf(R4, 8 * 512).rearrange("p (k n) -> p k n", k=8)
        wqm = P.bf(R4 + 2048, 8 * 256).rearrange("p (k n) -> p k n", k=8)
        Twkv, Twqm = T("wkv"), T("wqm")
        P.transfer(Twq, [Twkv, Twqm], c["scr"], self.Tscr)
        P.wload(wkv, self.w_mem_kv[l], [], [Twkv])
        P.wload(wqm, W_in[:, 2564:2820], [], [Twqm])
        P.DMA("sp", mb, self.mem_bias[l].rearrange("h (c m) -> m h c", c=2), [], [Tmb], slow=True)
        for mt in range(2):
            st, Tst = stage[mt], Tstage[mt]
            P.DMA("sp", st, self.mem_d[mt * 128:(mt + 1) * 128, :], [], [Tst])
            for half in range(2):
                ps, Tps = P.ps()
                for q in range(4):
                    kc = half * 4 + q
                    P.TR(ps[:, q * 128:(q + 1) * 128], st[:, kc * 128:(kc + 1) * 128], c["ident_f"], [Tst, self.Tc], [Tps])
                P.CP("act", memT[:, half * 4:(half + 1) * 4, mt * 128:(mt + 1) * 128], ps.rearrange("p (q t) -> p q t", q=4), [Tps], [TmemT])
        for ch in range(2):
            ps, Tps = P.ps()
            for kc in range(8):
                P.MM(ps[:, 0:256], wkv[:, kc, ch * 128:(ch + 1) * 128], memT[:, kc, :], kc == 0, kc == 7, [Twkv, TmemT], [Tps])
            P.CP("act", kmT[:, ch, :], ps[:, 0:256], [Tps], [Tkm])
        for mc in range(2):
            ps, Tps = P.ps()
            for kc in range(8):
                P.MM(ps[:, 0:256], memT[:, kc, mc * 128:(mc + 1) * 128], wkv[:, kc, 256:512], kc == 0, kc == 7, [Twkv, TmemT], [Tps])
            P.CP("dve", vm[:, mc, :], ps[:, 0:256], [Tps], [Tvm])
        for ch in range(2):
            for tg in range(4):
                ps, Tps = P.ps()
                for kc in range(8):
                    P.MM(ps, wqm[:, kc, ch * 128:(ch + 1) * 128], xT[:, kc, tg * 512:(tg + 1) * 512], kc == 0, kc == 7, [Twqm, TxT], [Tps])
                P.ACT(qmT[:, ch, tg * 512:(tg + 1) * 512], ps, AF.Copy, [Tps], [Tqm], scale=0.125)
        ui = 0
        for ch in range(2):
            for tg in range(4):
                psn, Tpsn = P.ps()
                psd, Tpsd = P.ps()
                for hh in range(2):
                    h = ch * 2 + hh
                    rows = slice(hh * 64, hh * 64 + 64)
                    pm, Tpm = Pm[ui % 2], TPm[ui % 2]
                    for mc in range(2):
                        ps, Tps = P.ps()
                        P.MM(ps, kmT[rows, ch, mc * 128:(mc + 1) * 128], qmT[rows, ch, tg * 512:(tg + 1) * 512], True, True, [Tkm, Tqm], [Tps])
                        P.ACT(pm[mc], ps, AF.Exp, [Tps, Tmb], [Tpm], bias=mb[:, h, mc:mc + 1])
                    for mc in range(2):
                        P.MM(psn[rows, :], vm[:, mc, h * 64:(h + 1) * 64], pm[mc], mc == 0, mc == 1, [Tvm, Tpm], [Tpsn])
                    for mc in range(2):
                        P.MM(psd[rows, :], c["ones_b"][:, 0:64], pm[mc], mc == 0, mc == 1, [self.Tc, Tpm], [Tpsd])
                    ui += 1
                P.RECIP(rec, psd, [Tpsd], [Trec])
                P.TT("dve", mixT[:, 6 + ch, tg * 512:(tg + 1) * 512], psn, rec, ALU.mult, [Tpsn, Trec], [Tmix])

        if self.stop == "mem":
            Td = T("dbgmix")
            P.DMA("sp", self.dbg_mix, mixT.rearrange("p k t -> p (k t)"), [Tmix], [Td])
            self.final.append(Td)
            return
        old = [TmemT, Tkm, Tvm, Tqm, Trec, Tmb] + TPm
        o = R3
        xbcT = P.bf(o, 6 * TOK).rearrange("p (k t) -> p k t", k=6); o += 3 * TOK
        XB = P.bf(o, 16 * 512).rearrange("p (j c) -> p j c", j=16); o += 16 * 256
        stg = [P.f32(o, 515), P.f32(o + 516, 515)]; o += 1032
        ctmp = P.f32(o, 512); o += 512
        cact = P.bf(o, 512); o += 256
        halo = P.f32(o, 18).rearrange("p (k c) -> p k c", k=6); o += 18
        cw = P.f32(o, 24).rearrange("p (k c) -> p k c", k=4); o += 24
        cb = P.f32(o, 6); o += 6
        dtb = P.f32(o, 4); o += 4
        Ab = P.f32(o, 4); o += 4
        dsk = P.f32(o, 4); o += 4
        nw = P.f32(o, 2); o += 2
        dt_sb = P.f32(o, 128); o += 128
        adt = P.f32(o, 128); o += 128
        cs = P.f32(o, 128); o += 128
        dec = P.f32(o, 128); o += 128
        ecs = P.f32(o, 128); o += 128
        dc = P.f32(o, 128); o += 128
        dtdec = P.f32(o, 128); o += 128
        Hst = P.f32(o, 256); o += 256
        Htmp = P.f32(o, 256); o += 256
        Hb = P.bf(o, 256); o += 128
        Xdt = P.bf(o, 256); o += 128
        Xdd = P.bf(o, 256); o += 128
        amask = P.f32(o, 512); o += 512
        expD = P.f32(o, 512); o += 512
        Msb = P.bf(o, 512); o += 256
        zs = P.f32(o, 256); o += 256
        y1 = P.f32(o, 256); o += 256
        y2 = P.f32(o, 256); o += 256
        ynb = P.bf(o, 256); o += 128
        ss = P.f32(o, 2); o += 2
        assert o <= R3 + 16384, o - R3
        names = "xbcT XB stg0 stg1 ctmp cact halo par dt adt cs dec ecs dc dtdec H Htmp Hb Xdt Xdd amask expD M zs y1 y2 ynb ss".split()
        tt_ = {n: T(n) for n in names}
        P.transfer(old, list(tt_.values()), c["scr"], self.Tscr)
        wx = [P.bf(R4 + i * 512, 8 * 128).rearrange("p (k n) -> p k n", k=8) for i in range(2)]
        Twx = [T("wx0"), T("wx1")]
        wz = P.bf(R4 + 1024, 8 * 256).rearrange("p (k n) -> p k n", k=8)
        wdt = P.bf(R4 + 2048, 8 * 4).rearrange("p (k n) -> p k n", k=8)
        Twz, Twdt = T("wz"), T("wdt")
        P.transfer([Twkv, Twqm], Twx + [Twz, Twdt], c["scr"], self.Tscr)
        Tpar = tt_["par"]
        P.wload(wz, W_in[:, 1536:1792], [], [Twz])
        P.wload(wdt, W_in[:, 2560:2564], [], [Twdt])
        P.DMA("sp", cw, self.conv_w[l].rearrange("k (c p) -> p k c", p=128), [], [Tpar], slow=True)
        P.DMA("sp", cb, self.conv_b[l:l + 1, :].rearrange("o (c p) -> p (o c)", p=128), [], [Tpar], slow=True)
        P.DMA("sp", dtb, self.dt_bias[l:l + 1, :].partition_broadcast(128), [], [Tpar])
        P.DMA("sp", Ab, self.a_log[l:l + 1, :].partition_broadcast(128), [], [Tpar])
        P.DMA("sp", dsk, self.d_skip[l:l + 1, :].partition_broadcast(128), [], [Tpar])
        P.DMA("sp", nw, self.ssd_nw[l:l + 1, :].rearrange("o (c p) -> p (o c)", p=128), [], [Tpar], slow=True)
        P.ACT(Ab, Ab, AF.Exp, [Tpar], [Tpar])
        P.TS("dve", Ab, Ab, -1.0, None, ALU.mult, None, [Tpar], [Tpar])
        pl, Tpl = self.plong
        for ti in range(32):
            src_xT, Tsrc = (xTp, TxTp) if ti < 16 else (xT, TxT)
            tl = ti % 16
            for kc in range(8):
                P.MM(pl[:, ti * 4:(ti + 1) * 4], src_xT[:, kc, tl * 128:(tl + 1) * 128], wdt[:, kc, :], kc == 0, kc == 7, [Twdt, Tsrc], [Tpl])
        v3 = lambda ap: ap.rearrange("p (t h) -> p t h", h=4)
        bc4 = lambda ap: ap.unsqueeze(1).to_broadcast([128, 32, 4])
        P.TT("dve", v3(dt_sb), v3(pl[:, 0:128]), bc4(dtb), ALU.add, [Tpl, Tpar], [tt_["dt"]])
        P.ACT(dt_sb, dt_sb, AF.Exp, [tt_["dt"]], [tt_["dt"]])
        P.ACT(dt_sb, dt_sb, AF.Ln, [tt_["dt"]], [tt_["dt"]], bias=1.0)
        P.TT("dve", v3(adt), v3(dt_sb), bc4(Ab), ALU.mult, [tt_["dt"], Tpar], [tt_["adt"]])
        P.MM(pl[:, 128:256], c["tri"], adt, True, True, [self.Tc, tt_["adt"]], [Tpl])
        P.MM(pl[:, 256:384], c["ones_f"], adt, True, True, [self.Tc, tt_["adt"]], [Tpl])
        P.CP("act", cs, pl[:, 128:256], [Tpl], [tt_["cs"]])
        P.TT("dve", dec, pl[:, 256:384], cs, ALU.subtract, [Tpl, tt_["cs"]], [tt_["dec"]])
        P.ACT(dec, dec, AF.Exp, [tt_["dec"]], [tt_["dec"]])
        P.ACT(ecs, cs, AF.Exp, [tt_["cs"]], [tt_["ecs"]])
        P.ACT(dc, pl[:, 256:384], AF.Exp, [Tpl], [tt_["dc"]])
        P.TT("dve", dtdec, dt_sb, dec, ALU.mult, [tt_["dt"], tt_["dec"]], [tt_["dtdec"]])
        P.MEMSET("pool", Hst, 0.0, [], [tt_["H"]])
        P.MEMSET("pool", halo, 0.0, [], [tt_["halo"]])

        bc64 = lambda ap: ap.unsqueeze(2).to_broadcast([128, 4, 64])
        v4 = lambda ap: ap.rearrange("p (h e) -> p h e", h=4)
        wxi = [0]
        ctmp2 = [ctmp, P.f32(R4 + 2064, 512)]
        Tctmp2 = [tt_["ctmp"], T("ctmp_b")]
        P.transfer([Twkv, Twqm], [Tctmp2[1]], c["scr"], self.Tscr)

        def conv_pass(cc, src_xT, Tsrc, tgs, own):
            wbuf, Twb_ = wx[wxi[0] % 2], Twx[wxi[0] % 2]
            wxi[0] += 1
            P.wload(wbuf, W_in[:, 1792 + cc * 128:1792 + (cc + 1) * 128], [], [Twb_])
            def proj(tg):
                ps, Tps = P.ps()
                for kc in range(8):
                    P.MM(ps, wbuf[:, kc, :], src_xT[:, kc, tg * 512:(tg + 1) * 512], kc == 0, kc == 7, [Twb_, Tsrc], [Tps])
                return ps, Tps

            def stage_a(gi, ps, Tps):
                sg, Tsg = stg[gi % 2], tt_["stg%d" % (gi % 2)]
                ct, Tct = ctmp2[gi % 2], Tctmp2[gi % 2]
                P.CP("dve", sg[:, 0:3], halo[:, cc, :], [tt_["halo"]], [Tsg])
                P.CP("act", sg[:, 3:515], ps, [Tps], [Tsg])
                P.CP("dve", halo[:, cc, :], sg[:, 512:515], [Tsg], [tt_["halo"]])
                P.ACT(ct, sg[:, 3:515], AF.Identity, [Tsg, Tpar], [Tct], scale=cw[:, 3, cc:cc + 1], bias=cb[:, cc:cc + 1])

            def stage_b(gi, tg):
                sg, Tsg = stg[gi % 2], tt_["stg%d" % (gi % 2)]
                ct, Tct = ctmp2[gi % 2], Tctmp2[gi % 2]
                for k in range(3):
                    P.STT("dve", ct, sg[:, k:k + 512], cw[:, k, cc:cc + 1], ct, ALU.mult, ALU.add, [Tsg, Tpar, Tct], [Tct])
                if own:
                    dst = xbcT[:, cc, tg * 512:(tg + 1) * 512]; Tdst = tt_["xbcT"]
                else:
                    dst = cact; Tdst = tt_["cact"]
                P.ACT(dst, ct, AF.Silu, [Tct], [Tdst])
                if cc < 4:
                    pb_, Tpb_ = self.pbf
                    for q in range(4):
                        P.TR(pb_[:, q * 128:(q + 1) * 128], dst[:, q * 128:(q + 1) * 128], c["ident_b"], [Tdst, self.Tc], [Tpb_])
                    P.CP("dve", XB[:, tg * 4:(tg + 1) * 4, cc * 128:(cc + 1) * 128], pb_[:, 0:512].rearrange("p (j c) -> p j c", j=4), [Tpb_], [tt_["XB"]])

            n_ = len(tgs)
            pj = {0: proj(tgs[0])}
            if n_ > 1:
                pj[1] = proj(tgs[1])
            stage_a(0, *pj.pop(0))
            for gi, tg in enumerate(tgs):
                if gi + 2 < n_:
                    pj[gi + 2] = proj(tgs[gi + 2])
                if gi + 1 < n_:
                    stage_a(gi + 1, *pj.pop(gi + 1))
                stage_b(gi, tg)

        o2 = R5
        zs2 = [zs, P.f32(o2, 256)]; o2 += 256
        amask2 = [amask, P.f32(o2, 512)]; o2 += 512
        expD2 = [expD, P.f32(o2, 512)]; o2 += 512
        Msb2 = [Msb, P.bf(o2, 512)]; o2 += 256
        Xdt2 = [Xdt, P.bf(o2, 256)]; o2 += 128
        Xdd2 = [Xdd, P.bf(o2, 256)]; o2 += 128
        assert o2 <= R5 + 2048
        names2 = "zs amask expD M Xdt Xdd".split()
        tt2 = {n: [tt_[n], T(n + "_b")] for n in names2}
        P.transfer(Tstage, [tt2[n][1] for n in names2], c["scr"], self.Tscr)

        def state_front(ti, par):
            tl = ti % 16
            col = slice(ti * 4, ti * 4 + 4)
            xdd, Txdd = Xdd2[par], tt2["Xdd"][par]
            P.TT("dve", v4(xdd), v4(XB[:, tl, 0:256]), bc64(dtdec[:, col]), ALU.mult, [tt_["XB"], tt_["dtdec"]], [Txdd])
            ps, Tps = P.ps()
            for g in range(2):
                P.MM(ps[:, g * 128:(g + 1) * 128], XB[:, tl, 256 + g * 128:256 + (g + 1) * 128], xdd[:, g * 128:(g + 1) * 128], True, True,
                     [tt_["XB"], Txdd], [Tps])
            return ps, Tps

        def state_back(ti, ps, Tps):
            col = slice(ti * 4, ti * 4 + 4)
            P.TT("dve", v4(Htmp), v4(Hst), bc64(dc[:, col]), ALU.mult, [tt_["H"], tt_["dc"]], [tt_["Htmp"]])
            P.TT("dve", Hst, Htmp, ps[:, 0:256], ALU.add, [tt_["Htmp"], Tps], [tt_["H"]])

        for cc in range(4):
            conv_pass(cc, xTp, TxTp, [0, 1, 2, 3], False)
        for cc in (4, 5):
            conv_pass(cc, xTp, TxTp, [3], False)
        pend = state_front(0, 0)
        for ti in range(16):
            nxt = state_front(ti + 1, (ti + 1) % 2) if ti + 1 < 16 else None
            state_back(ti, *pend)
            pend = nxt
        P.TS("dve", Hst, Hst, c["flag"][:, 0:1], None, ALU.mult, None, [tt_["H"], self.Tflag], [tt_["H"]])
        P.TS("dve", halo.rearrange("p k c -> p (k c)"), halo.rearrange("p k c -> p (k c)"), c["flag"][:, 0:1], None, ALU.mult, None,
             [tt_["halo"], self.Tflag], [tt_["halo"]])
        for cc in range(6):
            conv_pass(cc, xT, TxT, [0, 1, 2, 3], True)

        def own_front1(tl, par):
            ti = 16 + tl
            col = slice(ti * 4, ti * 4 + 4)
            tok = slice(tl * 128, (tl + 1) * 128)
            ps1, Tps1 = P.ps()
            for kc in range(8):
                P.MM(ps1[:, 0:256], xT[:, kc, tok], wz[:, kc, :], kc == 0, kc == 7, [Twz, TxT], [Tps1])
            for g in range(2):
                P.MM(ps1[:, 256 + g * 128:256 + (g + 1) * 128], xbcT[:, 2 + g, tok], xbcT[:, 4 + g, tok], True, True, [tt_["xbcT"]], [Tps1])
            P.ACT(zs2[par], ps1[:, 0:256], AF.Exp, [Tps1], [tt2["zs"][par]], scale=-1.0)
            am, Tam = amask2[par], tt2["amask"][par]
            psD, TpsD = P.ps()
            for h in range(4):
                P.TS("dve", am[:, h * 128:(h + 1) * 128], c["su"], adt[:, ti * 4 + h:ti * 4 + h + 1], None, ALU.mult, None,
                     [self.Tc, tt_["adt"]], [Tam])
            for h in range(4):
                P.MM(psD[:, h * 128:(h + 1) * 128], am[:, h * 128:(h + 1) * 128], c["tri"], True, False, [Tam, self.Tc], [TpsD])
                P.MM(psD[:, h * 128:(h + 1) * 128], c["ident_f"], c["neg"], False, True, [self.Tc], [TpsD])
            P.ACT(expD2[par], psD, AF.Exp, [TpsD], [tt2["expD"][par]])
            return ps1, Tps1

        def own_front2(tl, par, f1):
            ti = 16 + tl
            col = slice(ti * 4, ti * 4 + 4)
            ps1, Tps1 = f1
            P.TS("dve", zs2[par], zs2[par], 1.0, None, ALU.add, None, [tt2["zs"][par]], [tt2["zs"][par]])
            P.RECIP(zs2[par], zs2[par], [tt2["zs"][par]], [tt2["zs"][par]])
            P.TT("dve", zs2[par], ps1[:, 0:256], zs2[par], ALU.mult, [Tps1, tt2["zs"][par]], [tt2["zs"][par]])
            P.TT("dve", Msb2[par].rearrange("p (g h l) -> p g h l", g=2, h=2), expD2[par].rearrange("p (g h l) -> p g h l", g=2, h=2),
                 ps1[:, 256:512].rearrange("p (g l) -> p g l", g=2).unsqueeze(2).to_broadcast([128, 2, 2, 128]), ALU.mult,
                 [tt2["expD"][par], Tps1], [tt2["M"][par]])
            P.TT("dve", v4(Xdt2[par]), v4(XB[:, tl, 0:256]), bc64(dt_sb[:, col]), ALU.mult, [tt_["XB"], tt_["dt"]], [tt2["Xdt"][par]])
            return state_front(ti, par) if tl < 15 else None

        def own_back(tl, par, st):
            ti = 16 + tl
            col = slice(ti * 4, ti * 4 + 4)
            tok = slice(tl * 128, (tl + 1) * 128)
            msb, Tmsb = Msb2[par], tt2["M"][par]
            xdt, Txdt = Xdt2[par], tt2["Xdt"][par]
            P.CP("pool", Hb, Hst, [tt_["H"]], [tt_["Hb"]])
            psY, TpsY = P.ps()
            for h in range(4):
                P.MM(psY[:, h * 64:(h + 1) * 64], msb[:, h * 128:(h + 1) * 128], xdt[:, h * 64:(h + 1) * 64], True, True, [Tmsb, Txdt], [TpsY])
            for g in range(2):
                P.MM(psY[:, 256 + g * 128:256 + (g + 1) * 128], xbcT[:, 4 + g, tok], Hb[:, g * 128:(g + 1) * 128], True, True, [tt_["xbcT"], tt_["Hb"]], [TpsY])
            P.TT("dve", v4(y1), v4(psY[:, 256:512]), bc64(ecs[:, col]), ALU.mult, [TpsY, tt_["ecs"]], [tt_["y1"]])
            P.TT("dve", y1, y1, psY[:, 0:256], ALU.add, [tt_["y1"], TpsY], [tt_["y1"]])
            P.TT("dve", v4(y2), v4(XB[:, tl, 0:256]), bc64(dsk), ALU.mult, [tt_["XB"], Tpar], [tt_["y2"]])
            P.TT("dve", y1, y1, y2, ALU.add, [tt_["y1"], tt_["y2"]], [tt_["y1"]])
            P.TT("dve", y1, y1, zs2[par], ALU.mult, [tt_["y1"], tt2["zs"][par]], [tt_["y1"]])
            P.MEMSET("dve", ss[:, 0:1], 0.0, [], [tt_["ss"]])
            P.ACT(y2, y1, AF.Square, [tt_["y1"], tt_["y2"], tt_["ss"]], [tt_["y2"], tt_["ss"]], accum_out=ss[:, 0:1], scale=1.0 / 16.0)
            P.ACT(ss[:, 1:2], ss[:, 0:1], AF.Ln, [tt_["ss"]], [tt_["ss"]], bias=RMS_EPS)
            P.ACT(ss[:, 1:2], ss[:, 1:2], AF.Exp, [tt_["ss"]], [tt_["ss"]], scale=-0.5)
            P.TS("dve", ynb, y1, ss[:, 1:2], None, ALU.mult, None, [tt_["y1"], tt_["ss"]], [tt_["ynb"]])
            pb_, Tpb_ = self.pbf
            for q in range(2):
                P.TR(pb_[:, q * 128:(q + 1) * 128], ynb[:, q * 128:(q + 1) * 128], c["ident_b"], [tt_["ynb"], self.Tc], [Tpb_])
            for q in range(2):
                P.ACT(mixT[:, 4 + q, tok], pb_[:, q * 128:(q + 1) * 128], AF.Identity, [Tpb_, Tpar], [Tmix], scale=nw[:, q:q + 1])
            if st is not None:
                state_back(ti, *st)

        pend = own_front2(0, 0, own_front1(0, 0))
        for tl in range(16):
            f1 = own_front1(tl + 1, (tl + 1) % 2) if tl + 1 < 16 else None
            own_back(tl, tl % 2, pend)
            pend = own_front2(tl + 1, (tl + 1) % 2, f1) if tl + 1 < 16 else None
        P.transfer([tt2[n][1] for n in names2], Tstage, c["scr"], self.Tscr)

        if self.debug:
            Td = T("dbgmix")
            P.DMA("sp", self.dbg_mix, mixT.rearrange("p k t -> p (k t)"), [Tmix], [Td])
            self.final.append(Td)

        if self.stop == "ssd":
            return
        old = list(tt_.values())
        acc = P.f32(R3, 16 * 1024).rearrange("p (j c) -> p j c", j=16)
        Tacc2 = [T("acc%d" % j) for j in range(16)]
        P.transfer(old, Tacc2, c["scr"], self.Tscr)
        wout = P.bf(R4, 8 * 1024).rearrange("p (k n) -> p k n", k=8)
        Twout = T("wout")
        P.transfer(Twx + [Twz, Twdt, Tctmp2[1]], [Twout], c["scr"], self.Tscr)
        P.wload(wout, self.w_out[l], [], [Twout])
        o = R1
        lng = P.f32(o, 1024); o += 1024
        lnb = P.f32(o, 1024); o += 1024
        lo = P.bf(o, 8 * 128).rearrange("p (k t) -> p k t", k=8); o += 512
        wr_f = P.f32(o, 128).rearrange("p (k n) -> p k n", k=8); o += 128
        wr_hi = P.bf(o, 128).rearrange("p (k n) -> p k n", k=8); o += 64
        wr_lo = P.bf(o, 128).rearrange("p (k n) -> p k n", k=8); o += 64
        rsb = [P.f32(o, 1024), P.f32(o + 1024, 1024)]; o += 2048
        bst = P.f32(o, 12); o += 12
        mv = P.f32(o, 4); o += 4
        Tln, Tlo, Twr = T("ln1"), T("lo"), T("wr")
        Trsb = [T("rsb0"), T("rsb1")]
        Tbst = T("bst")
        P.transfer([TxTp], [Tln, Tlo, Twr, Tbst] + Trsb, c["scr"], self.Tscr)
        P.DMA("sp", lng, self.ln1_g[l:l + 1, :].partition_broadcast(128), [], [Tln])
        P.DMA("sp", lnb, self.ln1_b[l:l + 1, :].partition_broadcast(128), [], [Tln])
        P.DMA("sp", wr_f, self.w_router.rearrange("(k p) n -> p k n", p=128), [], [Twr])
        P.CP("dve", wr_hi, wr_f, [Twr], [Twr])
        P.TT("dve", wr_f, wr_f, wr_hi, ALU.subtract, [Twr], [Twr])
        P.CP("dve", wr_lo, wr_f, [Twr], [Twr])
        gates = P.f32(small, 256).rearrange("p (t e) -> p t e", t=16)
        gtmp = [P.f32(small + 256 + i * 64, 64) for i in range(4)]
        Tg = T("gates")
        x1T = xT
        Tx1T = [T("x1T%d" % g) for g in range(4)]
        P.transfer([TxT], Tx1T, c["scr"], self.Tscr)
        pl, Tpl = self.plong

        def layer_norm(r, Tr, g_ap, b_ap, Tgb, out_ap, Tout_list, bst, mv, Tbst):
            P.BNS(bst[:, 0:6], r[:, 0:512], [Tr], [Tbst])
            P.BNS(bst[:, 6:12], r[:, 512:1024], [Tr], [Tbst])
            P.BNA(mv[:, 0:2], bst.rearrange("p (a b) -> p a b", a=2), [Tbst], [Tbst])
            P.ACT(mv[:, 2:3], mv[:, 1:2], AF.Sqrt, [Tbst], [Tbst], bias=LN_EPS)
            P.RECIP(mv[:, 2:3], mv[:, 2:3], [Tbst], [Tbst])
            P.STT("dve", r, r, mv[:, 0:1], g_ap, ALU.subtract, ALU.mult, [Tr, Tbst, Tgb], [Tr])
            P.STT("dve", out_ap, r, mv[:, 2:3], b_ap, ALU.mult, ALU.add, [Tr, Tbst, Tgb], Tout_list)

        wps = {}

        def wout_mm(tt):
            tok = slice(tt * 128, (tt + 1) * 128)
            st, Tst = stage[tt % 2], Tstage[tt % 2]
            x_ap, x_R = x_own(tt)
            P.DMA("sp", st, x_ap, x_R, [Tst])
            wps[tt] = []
            for half in range(2):
                ps, Tps = P.ps()
                for kc in range(8):
                    P.MM(ps, mixT[:, kc, tok], wout[:, kc, half * 512:(half + 1) * 512], kc == 0, kc == 7, [Tmix, Twout], [Tps])
                wps[tt].append((ps, Tps))

        def wout_res(tt):
            st, Tst = stage[tt % 2], Tstage[tt % 2]
            r, Tr = rsb[tt % 2], Trsb[tt % 2]
            for half, (ps, Tps) in enumerate(wps.pop(tt)):
                P.STT("dve", r[:, half * 512:(half + 1) * 512], st[:, half * 512:(half + 1) * 512], ALPHA, ps, ALU.mult, ALU.add, [Tst, Tps], [Tr])

        wout_mm(0)
        wout_res(0)
        for tt in range(NT):
            tok = slice(tt * 128, (tt + 1) * 128)
            r, Tr = rsb[tt % 2], Trsb[tt % 2]
            if tt + 1 < NT:
                wout_mm(tt + 1)
            layer_norm(r, Tr, lng, lnb, Tln, r, [Tr], bst, mv, Tbst)
            if tt + 1 < NT:
                wout_res(tt + 1)
            if self.debug:
                Td = T("dbgx1_%d" % tt)
                P.DMA("sp", self.dbg_x1[tok, :], r, [Tr], [Td])
                self.final.append(Td)
            P.ACT(acc[:, tt, :], r, AF.Copy, [Tr], [Tacc2[tt]], scale=ALPHA)
            for half in range(2):
                ps, Tps = P.ps()
                for q in range(4):
                    kc = half * 4 + q
                    P.TR(ps[:, q * 128:(q + 1) * 128], r[:, kc * 128:(kc + 1) * 128], c["ident_f"], [Tr, self.Tc], [Tps])
                hi_ap = x1T[:, half * 4:(half + 1) * 4, tok]
                psv = ps.rearrange("p (q t) -> p q t", q=4)
                P.CP("act", hi_ap, psv, [Tps], [Tx1T[tt // 4]])
                P.TT("dve", lo[:, half * 4:(half + 1) * 4, :], psv, hi_ap, ALU.subtract, [Tps, Tx1T[tt // 4]], [Tlo])
            k = 0
            for (a_, w_) in ((0, wr_hi), (1, wr_hi), (0, wr_lo)):
                for kc in range(8):
                    lhs = x1T[:, kc, tok] if a_ == 0 else lo[:, kc, :]
                    P.MM(pl[:, tt * 16:(tt + 1) * 16], lhs, w_[:, kc, :], k == 0, k == 23, [Tx1T[tt // 4], Tlo, Twr], [Tpl])
                    k += 1
        brt = P.f32(small + 512, 16)
        Tbrt = T("brt")
        P.DMA("sp", brt, self.b_router.partition_broadcast(128), [], [Tbrt])
        g3 = gates
        sc = P.f32(small + 528, 0) if False else None
        lg = P.f32(R1 + 6000, 256).rearrange("p (t e) -> p t e", t=16)
        t1 = P.f32(R1 + 6256, 256).rearrange("p (t e) -> p t e", t=16)
        t2 = P.f32(R1 + 6512, 256).rearrange("p (t e) -> p t e", t=16)
        m1 = P.f32(R1 + 6768, 64).rearrange("p (t g) -> p t g", t=16)
        m2 = P.f32(R1 + 6832, 64).rearrange("p (t g) -> p t g", t=16)
        gs = P.f32(R1 + 6896, 64).rearrange("p (t g) -> p t g", t=16)
        mx = P.f32(R1 + 6960, 16)
        Tgt = T("gtmp")
        P.transfer([], [Tgt], c["scr"], self.Tscr)
        P.TT("dve", lg, pl[:, 0:256].rearrange("p (t e) -> p t e", t=16), brt.unsqueeze(1).to_broadcast([128, 16, 16]), ALU.add, [Tpl, Tbrt], [Tgt])
        P.REDUCE(mx, lg, ALU.max, [Tgt], [Tgt])
        P.TT("dve", lg, lg, mx.unsqueeze(2).to_broadcast([128, 16, 16]), ALU.subtract, [Tgt], [Tgt])
        P.ACT(lg, lg, AF.Exp, [Tgt], [Tgt])
        P.REDUCE(mx, lg, ALU.add, [Tgt], [Tgt])
        P.RECIP(mx, mx, [Tgt], [Tgt])
        P.TT("dve", lg, lg, mx.unsqueeze(2).to_broadcast([128, 16, 16]), ALU.mult, [Tgt], [Tgt])
        lg4 = lg.rearrange("p t (g k) -> p t g k", g=4)
        t14 = t1.rearrange("p t (g k) -> p t g k", g=4)
        t24 = t2.rearrange("p t (g k) -> p t g k", g=4)
        bcg = lambda ap: ap.unsqueeze(3).to_broadcast([128, 16, 4, 4])
        P.REDUCE(m1, lg4, ALU.max, [Tgt], [Tgt])
        P.TT("dve", t14, lg4, bcg(m1), ALU.is_ge, [Tgt], [Tgt])
        P.STT("dve", t14, t14, -4.0, lg4, ALU.mult, ALU.add, [Tgt], [Tgt])
        P.REDUCE(m2, t14, ALU.max, [Tgt], [Tgt])
        P.TT("dve", gs, m1, m2, ALU.add, [Tgt], [Tgt])
        P.REDUCE(mx, gs, ALU.max, [Tgt], [Tgt])
        P.TT("dve", m1, gs, mx.unsqueeze(2).to_broadcast([128, 16, 4]), ALU.is_ge, [Tgt], [Tgt])
        P.TT("dve", t24, lg4, bcg(m2), ALU.is_ge, [Tgt], [Tgt])
        P.TT("dve", t24, t24, bcg(m1), ALU.mult, [Tgt], [Tgt])
        P.TT("dve", t2, t2, lg, ALU.mult, [Tgt], [Tgt])
        P.RECIP(mx, mx, [Tgt], [Tgt])
        P.TT("dve", g3, t2, mx.unsqueeze(2).to_broadcast([128, 16, 16]), ALU.mult, [Tgt], [Tg])

        if self.stop == "gate":
            Td = T("dbgg")
            P.DMA("sp", self.out_d[0:128, 0:256], gates.rearrange("p t e -> p (t e)"), [Tg], [Td])
            self.final.append(Td)
            return
        gh = mixT
        Tgh = [T("gh%d" % g) for g in range(4)]
        P.transfer([Tmix], Tgh, c["scr"], self.Tscr)
        slots = [P.bf(R1, 8 * 1024).rearrange("p (k n) -> p k n", k=8),
                 P.bf(R1 + 4096, 8 * 1024).rearrange("p (k n) -> p k n", k=8),
                 P.bf(R4, 8 * 1024).rearrange("p (k n) -> p k n", k=8)]
        Tslot = [T("slot0"), T("slot1"), T("slot2")]
        P.transfer([Tln, Tlo, Twr, Tbst, Tgt] + Trsb, Tslot[0:2], c["scr"], self.Tscr)
        P.transfer([Twout], Tslot[2:3], c["scr"], self.Tscr)
        mi = 0
        for e_ in range(NE):
            mats = (self.w_gate[l, e_], self.w_up[l, e_], self.w_down[l, e_])
            sl = []
            for m_ in range(3):
                s_i = mi % 3
                mi += 1
                P.wload(slots[s_i], mats[m_], [], [Tslot[s_i]])
                sl.append((slots[s_i], Tslot[s_i]))
            (wg_, Twg_), (wu_, Twu_), (wd_, Twd_) = sl
            for tg in range(4):
                tk = slice(tg * 512, (tg + 1) * 512)
                for fc in range(8):
                    ps, Tps = P.ps()
                    for kc in range(8):
                        P.MM(ps, wg_[:, kc, fc * 128:(fc + 1) * 128], x1T[:, kc, tk], kc == 0, kc == 7, [Twg_, Tx1T[tg]], [Tps])
                    P.ACT(gh[:, fc, tk], ps, AF.Silu, [Tps], [Tgh[tg]])
            for tg in range(4):
                tk = slice(tg * 512, (tg + 1) * 512)
                for fc in range(8):
                    ps, Tps = P.ps()
                    for kc in range(8):
                        P.MM(ps, wu_[:, kc, fc * 128:(fc + 1) * 128], x1T[:, kc, tk], kc == 0, kc == 7, [Twu_, Tx1T[tg]], [Tps])
                    P.TT("dve", gh[:, fc, tk], ps, gh[:, fc, tk], ALU.mult, [Tps, Tgh[tg]], [Tgh[tg]])
            if e_ == NE - 1:
                lng2 = P.f32(R0, 1024)
                lnb2 = P.f32(R0 + 1024, 1024)
                Tln2 = T("ln2")
                P.transfer(Tx1T, [Tln2], c["scr"], self.Tscr)
                P.DMA("sp", lng2, self.ln2_g[l:l + 1, :].partition_broadcast(128), [], [Tln2])
                P.DMA("sp", lnb2, self.ln2_b[l:l + 1, :].partition_broadcast(128), [], [Tln2])
                ost = stage
                Tost = Tstage
                bst2 = P.f32(small + 532, 12)
                mv2 = P.f32(small + 548, 4)
                Tbst2 = T("bst2")
            for tt in range(NT):
                tok = slice(tt * 128, (tt + 1) * 128)
                for half in range(2):
                    ps, Tps = P.ps()
                    for fc in range(8):
                        P.MM(ps, gh[:, fc, tok], wd_[:, fc, half * 512:(half + 1) * 512], fc == 0, fc == 7, [Tgh[tt // 4], Twd_], [Tps])
                    a_ap = acc[:, tt, half * 512:(half + 1) * 512]
                    P.STT("dve", a_ap, ps, gates[:, tt, e_:e_ + 1], a_ap, ALU.mult, ALU.add, [Tps, Tg, Tacc2[tt]], [Tacc2[tt]])
                if e_ == NE - 1:
                    r = acc[:, tt, :]
                    layer_norm(r, Tacc2[tt], lng2, lnb2, Tln2, ost[tt % 2], [Tost[tt % 2]], bst2, mv2, Tbst2)
                    o_ap, To = out_d(tt)
                    P.DMA("sp", o_ap, ost[tt % 2], [Tost[tt % 2]], [To])
        self.apos = base

    def barrier(self):
        P = self
        c = self.c
        old = list(_ALLT)
        Tb = T("barrier")
        bar = c["bar"]
        self.S.op("pool", lambda e: e.memset(bar[:, 0:1], 0.0), [], [Tb] + old)
        P.ACT(bar[:, 1:2], bar[:, 0:1], AF.Copy, [Tb], [T("bar_act")])
        P.CP("dve", bar[:, 2:3], bar[:, 0:1], [Tb], [T("bar_dve")])
        P.DMA("sp", bar[:, 3:4], self.flag_d, [Tb], [T("bar_sp")])
        for i, (ps, Tps) in enumerate(self.pbanks):
            P.MM(ps[:, 0:1], c["ones_f"], bar[:, 0:1], True, True, [Tb, self.Tc], [Tps])
            P.CP("act", bar[:, 4 + i:5 + i], ps[:, 0:1], [Tps], [T("bar_ps%d" % i)])

    def build(self, layer_ids):
        self.final = []
        self.setup()
        nl = len(layer_ids)
        Tout = T("out")
        x_own = lambda tt: (self.x_own[tt * 128:(tt + 1) * 128, :], [])
        x_pre = lambda tt: (self.x_pre[tt * 128:(tt + 1) * 128, :], [])
        for i, l in enumerate(layer_ids):
            last = i == nl - 1
            if last:
                out_fn = lambda tt: (self.out_d[tt * 128:(tt + 1) * 128, :], Tout)
            else:
                Txm = [T("xmid%d_%d" % (i, cch)) for cch in range(4)]
                out_fn = (lambda Txm: (lambda tt: (self.xmid[tt * 128:(tt + 1) * 128, :], Txm[tt // 4])))(Txm)
            self.layer(l, x_own, x_pre, out_fn)
            if not last:
                Txg = [T("xg%d_%d" % (i, cch)) for cch in range(4)]
                for cch in range(4):
                    src = self.xmid[cch * 512:(cch + 1) * 512, :]
                    dst = self.xg[cch * 1024:(cch + 1) * 1024, :]
                    self.S.op("pool", (lambda src, dst: (lambda e: e.collective_compute(
                        "AllGather", ALU.bypass, replica_groups=[[0, 1], [2, 3], [4, 5], [6, 7]], ins=[src], outs=[dst])))(src, dst),
                        [Txm[cch]], [Txg[cch]], dma=True, inc=1)
                self.barrier()
                x_own = (lambda Txm: (lambda tt: (self.xmid[tt * 128:(tt + 1) * 128, :], [Txm[tt // 4]])))(Txm)
                x_pre = (lambda Txg: (lambda tt: (self.xg[(tt // 4) * 1024 + (tt % 4) * 128:(tt // 4) * 1024 + (tt % 4) * 128 + 128, :],
                                                  [Txg[tt // 4]])))(Txg)
        self.final.append(Tout)
        self.S.emit(final_tiles=self.final)
        return self.nc


_CACHE = {}


def _get_prog():
    if "fused" not in _CACHE:
        p = Prog(DEPTH)
        p.build(list(range(DEPTH)))
        _CACHE["fused"] = p
    return _CACHE["fused"]


def _core_inputs(inputs, oh):
    f = lambda a: np.ascontiguousarray(np.asarray(a, dtype=np.float32))
    x = f(inputs["x"])
    shared = dict(
        w_in=f(inputs["w_in"]), w_out=f(inputs["w_out"]), rel_bias=f(inputs["rel_bias"]), oh=oh,
        conv_w=f(inputs["conv_w"]), conv_b=f(inputs["conv_b"]), dt_bias=f(inputs["dt_bias"]),
        a_log=f(inputs["a_log"]), d_skip=f(inputs["d_skip"]), ssd_norm_w=f(inputs["ssd_norm_w"]),
        w_mem_kv=f(inputs["w_mem_kv"]), mem_bias=f(inputs["mem_bias"]),
        ln1_g=f(inputs["ln1_g"]), ln1_b=f(inputs["ln1_b"]), ln2_g=f(inputs["ln2_g"]), ln2_b=f(inputs["ln2_b"]),
        w_router=f(inputs["w_router"]), b_router=f(inputs["b_router"]).reshape(1, NE),
        w_gate=f(inputs["w_gate"]), w_up=f(inputs["w_up"]), w_down=f(inputs["w_down"]),
    )
    zeros = np.zeros((TOK, D), np.float32)
    maps = []
    for core in range(8):
        b, h = core // 2, core % 2
        m = dict(shared)
        m["x_own"] = np.ascontiguousarray(x[b, h * TOK:(h + 1) * TOK])
        m["x_pre"] = np.ascontiguousarray(x[b, 0:TOK]) if h == 1 else zeros
        m["flag"] = np.full((128, 1), float(h), np.float32)
        m["mem"] = f(inputs["mem"][b])
        maps.append(m)
    return maps


def kernel(**inputs):
    oh = _onehot_table()
    prog = _get_prog()
    maps = _core_inputs(inputs, oh)
    res = run_bass_kernel_spmd(prog.nc, maps, core_ids=list(range(8)))
    out = np.empty((4, 2 * TOK, D), np.float32)
    for core in range(8):
        b, h = core // 2, core % 2
        out[b, h * TOK:(h + 1) * TOK] = res.results[core]["x_out"]
    return out
```

```python
import math
import numpy as np
import concourse.bass as bass
import concourse.mybir as mybir
from concourse.bass_utils import run_bass_kernel_spmd

F32 = mybir.dt.float32
BF16 = mybir.dt.bfloat16
ALU = mybir.AluOpType
AF = mybir.ActivationFunctionType

D = 1024
TOK = 2048
NT = 16
DEPTH = 2
N_IN = 2820
ALPHA = (2 * DEPTH) ** 0.25
LN_EPS = 1e-5
RMS_EPS = 1e-5
NE = 16
BRANCH_D = (1, 4, 16)
LG = 384
NEG = -30000.0
SAME_ENG_SYNC = True


_ALLT = []


class T:
    __slots__ = ("name", "w", "r", "sem", "cnt")

    def __init__(self, name=""):
        self.name = name
        self.w = None
        self.r = {}
        self.sem = None
        self.cnt = 0
        _ALLT.append(self)


class Sched:
    ENGS = ("pe", "act", "dve", "pool", "sp")

    def __init__(self, nc):
        self.nc = nc
        self.ops = []

    def op(self, eng, fn, reads=(), writes=(), dma=False, inc=16):
        deps = set()
        for t in reads:
            if t.w is not None:
                deps.add(t.w)
        for t in writes:
            if t.w is not None:
                deps.add(t.w)
            deps.update(t.r.values())
        idx = len(self.ops)
        dt = None
        if dma:
            assert len(writes) == 1
            dt = writes[0]
        best = {}
        red = set()
        for d in deps:
            Dp = self.ops[d]
            if Dp["dma"]:
                red.add(d)
            elif best.get(Dp["eng"], -1) < d:
                best[Dp["eng"]] = d
        red.update(best.values())
        self.ops.append(dict(eng=eng, fn=fn, deps=red, dma=dma, dt=dt, sig=False, cnt=0, dinc=inc))
        for t in reads:
            if dma:
                t.r[("dma", idx)] = idx
            else:
                t.r[eng] = idx
        for t in writes:
            t.w = idx
            t.r = {}
        return idx

    def emit(self, final_tiles=()):
        nc = self.nc
        ops = self.ops
        fdeps = set()
        for t in final_tiles:
            if t.w is not None:
                fdeps.add(t.w)
        ops.append(dict(eng="sp", fn=None, deps=fdeps, dma=False, dt=None, sig=False, cnt=0, dinc=16))
        for i, o in enumerate(ops):
            for d in o["deps"]:
                Dp = ops[d]
                if Dp["dma"]:
                    Dp["sig"] = True
                elif Dp["eng"] != o["eng"] or o["dma"]:
                    Dp["sig"] = True
                elif o["eng"] != "pe" and SAME_ENG_SYNC:
                    Dp["sig"] = True
        for o in ops:
            if o["dma"]:
                o["sig"] = True
        semreg = {}
        esem = {e: nc.alloc_semaphore(name="prog_" + e) for e in self.ENGS}
        ecnt = {e: 0 for e in self.ENGS}
        for o in ops:
            if not o["sig"]:
                continue
            if o["dma"]:
                t = o["dt"]
                if t.sem is None:
                    if t.name in semreg:
                        prev = semreg[t.name]
                        t.sem, t.cnt = prev.sem, prev.cnt
                    else:
                        t.sem = nc.alloc_semaphore(name="dma_" + t.name)
                    semreg[t.name] = t
                t.cnt += o["dinc"]
                o["sem"] = t.sem
                o["cnt"] = t.cnt
                o["inc"] = o["dinc"]
            else:
                ecnt[o["eng"]] += 1
                o["sem"] = esem[o["eng"]]
                o["cnt"] = ecnt[o["eng"]]
                o["inc"] = 1
        per = {e: [] for e in self.ENGS}
        for i, o in enumerate(ops):
            per[o["eng"]].append(i)
        nwaits = [0]

        def run(engname, engine):
            waited = {}
            for i in per[engname]:
                o = ops[i]
                need = {}
                for d in o["deps"]:
                    Dp = ops[d]
                    if not Dp["sig"]:
                        continue
                    if (not Dp["dma"]) and Dp["eng"] == engname and not o["dma"]:
                        if engname == "pe" or not SAME_ENG_SYNC:
                            continue
                    s = Dp["sem"]
                    key = id(s)
                    if need.get(key, (None, 0))[1] < Dp["cnt"]:
                        need[key] = (s, Dp["cnt"])
                for key, (s, c) in need.items():
                    if waited.get(key, 0) >= c:
                        continue
                    engine.wait_ge(s, c)
                    nwaits[0] += 1
                    waited[key] = c
                if o["fn"] is None:
                    continue
                ins = o["fn"](engine)
                if o["sig"]:
                    ins.then_inc(o["sem"], o["inc"])

        with nc.Block() as block:
            @block.sync
            def _(e):
                run("sp", e)

            @block.tensor
            def _(e):
                run("pe", e)

            @block.scalar
            def _(e):
                run("act", e)

            @block.vector
            def _(e):
                run("dve", e)

            @block.gpsimd
            def _(e):
                run("pool", e)
        self.stats = dict(nops=len(ops), nwaits=nwaits[0], counts=dict(ecnt))


def _t5_bucket_np(dist):
    n = np.maximum(dist, 0)
    max_exact = 16
    nf = np.maximum(n, max_exact).astype(np.float32)
    val = np.log(nf / np.float32(max_exact)) / np.float32(math.log(2048 / max_exact)) * np.float32(32 - max_exact)
    large = max_exact + val.astype(np.float32).astype(np.int32)
    large = np.minimum(large, 31)
    return np.where(n < max_exact, n, large)


def _onehot_table():
    oh = np.zeros((32, 3 * LG), np.float32)
    steps = np.arange(0, 129)
    for bi, d in enumerate(BRANCH_D):
        b = _t5_bucket_np(steps * d)
        for s_, u in zip(steps, b):
            oh[int(u), bi * LG + int(s_) + 127] = 1.0
    return oh


class Prog:
    def __init__(self, nlayers, debug=False, stop=None):
        self.nl = nlayers
        self.debug = debug
        self.stop = stop
        nc = bass.Bass("TRN2", target_bir_lowering=False)
        del _ALLT[:]
        self.nc = nc
        self.S = Sched(nc)
        L = nlayers
        di = lambda name, shape: nc.dram_tensor(name, shape, F32, kind="ExternalInput").ap()
        self.x_own = di("x_own", [TOK, D])
        self.x_pre = di("x_pre", [TOK, D])
        self.flag_d = di("flag", [128, 1])
        self.mem_d = di("mem", [256, D])
        self.w_in = di("w_in", [L, D, N_IN])
        self.w_out = di("w_out", [L, D, D])
        self.rel_bias = di("rel_bias", [32, 8])
        self.oh_d = di("oh", [32, 3 * LG])
        self.conv_w = di("conv_w", [L, 4, 768])
        self.conv_b = di("conv_b", [L, 768])
        self.dt_bias = di("dt_bias", [L, 4])
        self.a_log = di("a_log", [L, 4])
        self.d_skip = di("d_skip", [L, 4])
        self.ssd_nw = di("ssd_norm_w", [L, 256])
        self.w_mem_kv = di("w_mem_kv", [L, D, 512])
        self.mem_bias = di("mem_bias", [L, 4, 256])
        self.ln1_g = di("ln1_g", [L, D])
        self.ln1_b = di("ln1_b", [L, D])
        self.ln2_g = di("ln2_g", [L, D])
        self.ln2_b = di("ln2_b", [L, D])
        self.w_router = di("w_router", [D, NE])
        self.b_router = di("b_router", [1, NE])
        self.w_gate = di("w_gate", [L, NE, D, D])
        self.w_up = di("w_up", [L, NE, D, D])
        self.w_down = di("w_down", [L, NE, D, D])
        self.out_d = nc.dram_tensor("x_out", [TOK, D], F32, kind="ExternalOutput").ap()
        self.escr = nc.dram_tensor("escr", [24, 128 * (LG + 1)], F32, kind="Internal").ap()
        self.xmid = nc.dram_tensor("xmid", [TOK, D], F32, kind="Internal").ap()
        self.xg = nc.dram_tensor("xg", [4 * 2 * 512, D], F32, kind="Internal").ap()
        if debug:
            self.dbg_mix = nc.dram_tensor("dbg_mix", [128, 8 * TOK], BF16, kind="ExternalOutput").ap()
            self.dbg_x1 = nc.dram_tensor("dbg_x1", [TOK, D], F32, kind="ExternalOutput").ap()
        self.arena = nc.alloc_sbuf_tensor("arena", [128, 51968], F32)
        self.apos = 0
        self.pbanks = [(nc.alloc_psum_tensor("pb%d" % i, [128, 512], F32)[:, :], T("pb%d" % i)) for i in range(6)]
        self.pi = 0
        self.pbf = (nc.alloc_psum_tensor("pbf", [128, 1024], BF16)[:, :], T("pbf"))
        self.plong = (nc.alloc_psum_tensor("plong", [128, 512], F32)[:, :], T("plong"))
        self.pb8 = self.pbanks + [self.plong, (self.pbf[0].bitcast(F32), self.pbf[1])]
        self.pi8 = 0

    def region(self, words):
        a = self.apos
        self.apos += words
        assert self.apos <= 51968, self.apos
        return a

    def f32(self, off, n):
        return self.arena[:, off:off + n]

    def bf(self, off_words, n_bf):
        assert n_bf % 2 == 0
        return self.arena[:, off_words:off_words + n_bf // 2].bitcast(BF16)

    def ps(self):
        p = self.pbanks[self.pi % 6]
        self.pi += 1
        return p

    def ps8(self):
        p = self.pb8[self.pi8 % 8]
        self.pi8 += 1
        return p

    def MM(self, out, lhsT, rhs, start, stop, R, W):
        self.S.op("pe", lambda e: e.matmul(out, lhsT=lhsT, rhs=rhs, start=start, stop=stop), R, W)

    def TR(self, out, in_, ident, R, W):
        self.S.op("pe", lambda e: e.transpose(out=out, in_=in_, identity=ident), R, W)

    def ACT(self, out, in_, func, R, W, **kw):
        self.S.op("act", lambda e: e.activation(out=out, in_=in_, func=func, **kw), R, W)

    def TT(self, eng, out, in0, in1, op, R, W):
        self.S.op(eng, lambda e: e.tensor_tensor(out=out, in0=in0, in1=in1, op=op), R, W)

    def TS(self, eng, out, in0, s1, s2, op0, op1, R, W):
        if op1 is None:
            self.S.op(eng, lambda e: e.tensor_scalar(out=out, in0=in0, scalar1=s1, scalar2=None, op0=op0), R, W)
        else:
            self.S.op(eng, lambda e: e.tensor_scalar(out=out, in0=in0, scalar1=s1, scalar2=s2, op0=op0, op1=op1), R, W)

    def STT(self, eng, out, in0, scalar, in1, op0, op1, R, W):
        self.S.op(eng, lambda e: e.scalar_tensor_tensor(out=out, in0=in0, scalar=scalar, in1=in1, op0=op0, op1=op1), R, W)

    def CP(self, eng, out, in_, R, W):
        if eng == "act":
            self.ACT(out, in_, AF.Copy, R, W)
        else:
            self.S.op(eng, lambda e: e.tensor_copy(out=out, in_=in_), R, W)

    def DMA(self, eng, out, in_, R, W, slow=False):
        if slow:
            self.S.op(eng, lambda e: e.dma_start(out=out, in_=in_, allow_slow_non_contiguous=True), R, W, dma=True)
        else:
            self.S.op(eng, lambda e: e.dma_start(out=out, in_=in_), R, W, dma=True)

    def RECIP(self, out, in_, R, W):
        self.S.op("dve", lambda e: e.reciprocal(out=out, in_=in_), R, W)

    def REDUCE(self, out, in_, op, R, W):
        self.S.op("dve", lambda e: e.tensor_reduce(out=out, in_=in_, axis=mybir.AxisListType.X, op=op), R, W)

    def BNS(self, out, in_, R, W):
        self.S.op("dve", lambda e: e.bn_stats(out=out, in_=in_), R, W)

    def BNA(self, out, in_, R, W):
        self.S.op("dve", lambda e: e.bn_aggr(out=out, in_=in_), R, W)

    def MEMSET(self, eng, ap, val, R, W):
        self.S.op(eng, lambda e: e.memset(ap, val), R, W)

    def ASEL(self, out, in_, pattern, cmp, fill, cm, R, W):
        self.S.op("pool", lambda e: e.affine_select(out=out, in_=in_, pattern=pattern, compare_op=cmp,
                                                     fill=fill, base=0, channel_multiplier=cm), R, W)

    def transfer(self, old, new, scratch, Tscr):
        self.S.op("pool", lambda e: e.memset(scratch, 0.0), [], [Tscr] + list(old) + list(new))

    def setup(self):
        P = self
        c = {}
        self.c = c
        off = P.region(128); c["ident_f"] = P.f32(off, 128)
        off = P.region(128); c["tri"] = P.f32(off, 128)
        off = P.region(128); c["su"] = P.f32(off, 128)
        off = P.region(128); c["neg"] = P.f32(off, 128)
        off = P.region(128); c["ones_f"] = P.f32(off, 128)
        off = P.region(64); c["ident_b"] = P.bf(off, 128)
        off = P.region(64); c["ones_b"] = P.bf(off, 128)
        off = P.region(32); c["flagm"] = P.bf(off, 64)
        off = P.region(2); c["flag"] = P.f32(off, 1); c["scr"] = P.f32(off + 1, 1)
        off = P.region(16); c["bar"] = P.f32(off, 16)
        off = P.region(3 * 8 * 128); c["etab"] = P.bf(off, 3 * 8 * 256).rearrange("p (b h c) -> p b h c", b=3, h=8)
        Tc = T("consts")
        self.Tc = Tc
        self.Tscr = T("scr")
        self.Tflag = T("flag")
        self.Tetab = T("etab")
        ones_f = c["ones_f"]
        P.MEMSET("pool", ones_f, 1.0, [], [Tc])
        P.MEMSET("pool", c["ones_b"], 1.0, [], [Tc])
        P.MEMSET("pool", c["scr"], 0.0, [], [self.Tscr])
        P.ASEL(c["ident_f"], ones_f, [[-1, 128]], ALU.is_equal, 0.0, 1, [Tc], [Tc])
        P.ASEL(c["tri"], ones_f, [[1, 128]], ALU.is_ge, 0.0, -1, [Tc], [Tc])
        P.ASEL(c["su"], ones_f, [[-1, 128]], ALU.is_gt, 0.0, 1, [Tc], [Tc])
        P.MEMSET("pool", c["neg"], NEG, [], [Tc])
        P.ASEL(c["neg"], c["neg"], [[-1, 128]], ALU.is_gt, 0.0, 1, [Tc], [Tc])
        P.CP("pool", c["ident_b"], c["ident_f"], [Tc], [Tc])
        P.DMA("sp", c["flag"], self.flag_d, [], [self.Tflag])
        P.TS("dve", c["flagm"], c["ones_b"][:, 0:64], c["flag"][:, 0:1], None, ALU.mult, None, [Tc, self.Tflag], [Tc])

    def build_etab(self, tmp_off):
        P = self
        c = self.c
        o = tmp_off
        rb = P.f32(o, 8)[0:32, :]; o += 8
        lh = P.f32(o, 128)[0:32, :]; o += 128
        oh = P.f32(o, 3 * LG)[0:32, :]; o += 3 * LG
        gsb = [P.f32(o + i * LG, LG) for i in range(2)]; o += 2 * LG
        Trb, Tlh, Toh = T("rb"), T("lh"), T("oh")
        Tg = [T("gsb0"), T("gsb1")]
        Tes2 = [T("escr0"), T("escr1")]
        P.DMA("sp", rb, self.rel_bias, [], [Trb])
        P.DMA("sp", oh, self.oh_d, [], [Toh])
        P.ACT(rb, rb, AF.Exp, [Trb], [Trb])
        k = 0
        for h in range(8):
            P.TS("dve", lh, c["ones_f"][0:32, :], rb[:, h:h + 1], None, ALU.mult, None, [self.Tc, Trb], [Tlh])
            for b in range(3):
                ps, Tps = P.ps()
                P.MM(ps[:, 0:LG], lh, oh[:, b * LG:(b + 1) * LG], True, True, [Tlh, Toh], [Tps])
                g, Tgk = gsb[k % 2], Tg[k % 2]
                P.CP("act", g, ps[:, 0:LG], [Tps], [Tgk])
                idx = h * 3 + b
                dst = self.escr[idx].rearrange("(p c) -> p c", c=LG + 1)[:, 0:LG]
                Tes = Tes2[k % 2]
                P.DMA("sp", dst, g, [Tgk], [Tes])
                src = bass.AP(tensor=self.escr.tensor, offset=idx * 128 * (LG + 1) + 127,
                              ap=[[LG, 128], [128, 2], [1, 128]])
                P.DMA("pool", c["etab"][:, b, h, :].rearrange("p (a q) -> p a q", a=2), src, [Tes], [self.Tetab])
                k += 1
                yield None
        return [Trb, Tlh, Toh] + Tg

    def load_xT(self, x_fn, xT, TxT, stage, Tstage):
        P = self
        c = self.c
        for tt in range(NT):
            st, Tst = stage[tt % 2], Tstage[tt % 2]
            x_ap, x_R = x_fn(tt)
            P.DMA("sp", st, x_ap, x_R, [Tst])
            for half in range(2):
                ps, Tps = P.ps()
                for q in range(4):
                    kc = half * 4 + q
                    P.TR(ps[:, q * 128:(q + 1) * 128], st[:, kc * 128:(kc + 1) * 128], c["ident_f"], [Tst, self.Tc], [Tps])
                P.CP("act" if half == 0 else "dve", xT[:, half * 4:(half + 1) * 4, tt * 128:(tt + 1) * 128],
                     ps.rearrange("p (q t) -> p q t", q=4), [Tps], [TxT])

    def wload(self, dst, src2d, R, W):
        self.DMA("pool", dst, src2d.rearrange("(k p) n -> p k n", p=128), R, W)

    def layer(self, l, x_own, x_pre, out_d):
        P = self
        c = self.c
        base = self.apos
        R0 = P.region(8192)
        R1 = P.region(8192)
        R2 = P.region(8192)
        R3 = P.region(16384)
        R4 = P.region(4096)
        R5 = P.region(2048 + 512 + 64)
        xT = P.bf(R0, 8 * TOK).rearrange("p (k t) -> p k t", k=8)
        xTp = P.bf(R1, 8 * TOK).rearrange("p (k t) -> p k t", k=8)
        mixT = P.bf(R2, 8 * TOK).rearrange("p (k t) -> p k t", k=8)
        stage = [P.f32(R5, 1024), P.f32(R5 + 1024, 1024)]
        Tstage = [T("stage0"), T("stage1")]
        small = R5 + 2048
        TxT, TxTp, Tmix = T("xT"), T("xTp"), T("mixT")
        W_in = self.w_in[l]

        wq = [P.bf(R4 + i * 1536, 3 * 8 * 128).rearrange("p (s k n) -> p s k n", s=3, k=8) for i in range(2)]
        Twq = [T("wq0"), T("wq1")]
        for s_ in range(3):
            P.wload(wq[0][:, s_], W_in[:, s_ * 512:s_ * 512 + 128], [], [Twq[0]])
        P.load_xT(x_own, xT, TxT, stage, Tstage)
        P.load_xT(x_pre, xTp, TxTp, stage, Tstage)
        etab_state = {"gen": self.build_etab(R2) if l == 0 else None, "tmp": []}

        def etab_step():
            g_ = etab_state["gen"]
            if g_ is None:
                return
            try:
                next(g_)
            except StopIteration as e_:
                etab_state["tmp"] = e_.value
                etab_state["gen"] = None

        if self.stop == "etab":
            Td = T("dbgmix")
            P.DMA("sp", self.dbg_mix[:, 0:6144], c["etab"].rearrange("p b h c -> p (b h c)"), [self.Tetab], [Td])
            self.final.append(Td)
            return
        if self.stop == "xT":
            Td = T("dbgmix")
            P.DMA("sp", self.dbg_mix, xT.rearrange("p k t -> p (k t)"), [TxT, TxTp], [Td])
            self.final.append(Td)
            return
        o = R3
        QT = []; KT = []; Vo = []; KpT = []; Vp = []
        for bi, d in enumerate(BRANCH_D):
            QT.append(P.bf(o, TOK)); o += TOK // 2
        for bi, d in enumerate(BRANCH_D):
            KT.append(P.bf(o, TOK)); o += TOK // 2
        npre = {1: 1, 4: 4, 16: 16}
        for bi, d in enumerate(BRANCH_D):
            KpT.append(P.bf(o, npre[d] * 128)); o += npre[d] * 64
        for bi, d in enumerate(BRANCH_D):
            Vo.append(P.bf(o, 16 * 128).rearrange("p (j c) -> p j c", j=16)); o += 16 * 64
        for bi, d in enumerate(BRANCH_D):
            Vp.append(P.bf(o, npre[d] * 128).rearrange("p (j c) -> p j c", j=npre[d])); o += npre[d] * 64
        accND = P.f32(o, 2 * TOK).rearrange("p (a t) -> p a t", a=2); o += 2 * TOK
        assert o <= R3 + 16384, o - R3
        Psb = [P.bf(R5, 512), P.bf(R5 + 1024, 512), P.bf(R5 + 256, 512), P.bf(R5 + 1280, 512)]
        TPsb = [T("Psb%d" % i) for i in range(4)]
        P.transfer(Tstage, TPsb, c["scr"], self.Tscr)
        TQ = [T("QT%d" % i) for i in range(3)]
        TK = [T("KT%d" % i) for i in range(3)]
        TKp = [T("KpT%d" % i) for i in range(3)]
        TV = [T("V%d" % i) for i in range(3)]
        TVp = [T("Vp%d" % i) for i in range(3)]
        Tacc = T("accND")
        tmpND = P.f32(R2 + 4096, 2 * TOK).rearrange("p (a t) -> p a t", a=2)
        Ttmp = T("tmpND")

        def evac_reorder(eng, dst_flat, ps, d, tg, R, W, scale=None):
            if d == 1:
                o_ap = dst_flat[:, tg * 512:(tg + 1) * 512]
                i_ap = ps
            else:
                o_ap = dst_flat.rearrange("p (r m) -> p r m", r=d)[:, :, tg * (512 // d):(tg + 1) * (512 // d)]
                i_ap = ps.rearrange("p (m r) -> p r m", r=d)
            if eng == "act":
                if scale is None:
                    P.ACT(o_ap, i_ap, AF.Copy, R, W)
                else:
                    P.ACT(o_ap, i_ap, AF.Copy, R, W, scale=scale)
            else:
                if scale is None:
                    P.CP("dve", o_ap, i_ap, R, W)
                else:
                    P.TS("dve", o_ap, i_ap, scale, None, ALU.mult, None, R, W)

        for hp in range(4):
            wb, Twb = wq[hp % 2], Twq[hp % 2]
            for s_ in range(3):
                col = s_ * 512 + hp * 128
                if hp > 0:
                    P.wload(wb[:, s_], W_in[:, col:col + 128], [], [Twb])
            for which in range(3):
                src_xT, Tsrc = (xT, TxT) if which < 2 else (xTp, TxTp)
                wsel = wb[:, 0] if which == 0 else wb[:, 1]
                for tg in range(4):
                    ps, Tps = P.ps()
                    for kc in range(8):
                        P.MM(ps, wsel[:, kc, :], src_xT[:, kc, tg * 512:(tg + 1) * 512], kc == 0, kc == 7, [Twb, Tsrc], [Tps])
                    for bi, d in enumerate(BRANCH_D):
                        eng = "act"
                        if which == 0:
                            evac_reorder(eng, QT[bi], ps, d, tg, [Tps], [TQ[bi]], scale=0.125)
                        elif which == 1:
                            evac_reorder(eng, KT[bi], ps, d, tg, [Tps], [TK[bi]])
                        else:
                            if d == 16:
                                evac_reorder(eng, KpT[bi], ps, d, tg, [Tps], [TKp[bi]])
                            elif d == 4 and tg == 3:
                                P.CP(eng, KpT[bi].rearrange("p (r m) -> p r m", r=4), ps.rearrange("p (m r) -> p r m", r=4), [Tps], [TKp[bi]])
                            elif d == 1 and tg == 3:
                                P.CP(eng, KpT[bi], ps[:, 384:512], [Tps], [TKp[bi]])
                    if hp == 0:
                        etab_step()
            if self.stop == "qk":
                return
            for bi, d in enumerate(BRANCH_D):
                nblk = 16 // d
                for own in (True, False):
                    src_xT, Tsrc = (xT, TxT) if own else (xTp, TxTp)
                    if own:
                        tiles = [(j, (j // nblk), (j % nblk)) for j in range(16)]
                    else:
                        tiles = [(r, r, nblk - 1) for r in range(d)]
                    dstV, TdV = (Vo[bi], TV[bi]) if own else (Vp[bi], TVp[bi])
                    for g0 in range(0, len(tiles), 4):
                        grp = tiles[g0:g0 + 4]
                        ps, Tps = P.ps()
                        for gi, (j, r, n) in enumerate(grp):
                            start = n * 128 * d + r
                            for kc in range(8):
                                P.MM(ps[:, gi * 128:(gi + 1) * 128], src_xT[:, kc, start:start + 127 * d + 1:d], wb[:, 2, kc, :],
                                     kc == 0, kc == 7, [Twb, Tsrc], [Tps])
                        ng = len(grp)
                        if own:
                            P.CP("act" if (g0 // 4) % 2 == 0 else "dve", dstV[:, g0:g0 + ng, :],
                                 ps[:, 0:ng * 128].rearrange("p (j c) -> p j c", j=ng), [Tps], [TdV])
                        else:
                            P.TS("dve", dstV[:, g0:g0 + ng, :], ps[:, 0:ng * 128].rearrange("p (j c) -> p j c", j=ng),
                                 c["flag"][:, 0:1], None, ALU.mult, None, [Tps, self.Tflag], [TdV])
                        if hp == 0:
                            etab_step()
            if self.stop == "proj":
                return
            if hp == 0:
                while etab_state["gen"] is not None:
                    etab_step()
                if etab_state["tmp"]:
                    P.transfer(etab_state["tmp"], [Tmix, Ttmp], c["scr"], self.Tscr)
                    etab_state["tmp"] = []
            units = []
            for bi, d in enumerate(BRANCH_D):
                if self.stop == "b0" and bi > 0:
                    break
                for j in range(16):
                    units.append((bi, d, j))
            ust = {}

            def stage_scores(i):
                bi, d, j = units[i]
                nblk = 16 // d
                r, n = j // nblk, j % nblk
                if n > 0:
                    pK = KT[bi][:, (j - 1) * 128:j * 128]; TpK = TK[bi]
                    pV = Vo[bi][:, j - 1, :]; TpV = TV[bi]
                    pden = c["ones_b"][:, 0:64]
                else:
                    pK = KpT[bi][:, r * 128:(r + 1) * 128]; TpK = TKp[bi]
                    pV = Vp[bi][:, r, :]; TpV = TVp[bi]
                    pden = c["flagm"][:, 0:64]
                pss = [P.ps8(), P.ps8()]
                for hh in range(2):
                    ps, Tps = pss[hh]
                    rows = slice(hh * 64, hh * 64 + 64)
                    qv = QT[bi][rows, j * 128:(j + 1) * 128]
                    P.MM(ps[:, 128:256], pK[rows], qv, True, True, [TpK, TQ[bi]], [Tps])
                    P.MM(ps[:, 0:128], KT[bi][rows, j * 128:(j + 1) * 128], qv, True, True, [TK[bi], TQ[bi]], [Tps])
                pb, Tpb = Psb[i % 3], TPsb[i % 3]
                for hh in range(2):
                    ps, Tps = pss[hh]
                    P.ACT(pb[:, hh * 256:(hh + 1) * 256], ps[:, 0:256], AF.Exp, [Tps], [Tpb])
                P.TT("dve", pb, pb, c["etab"][:, bi, 2 * hp:2 * hp + 2, :].rearrange("p h c -> p (h c)"), ALU.mult, [Tpb, self.Tetab], [Tpb])
                ust[i] = (pb, Tpb, pV, TpV, pden, r, n)

            def stage_pv(i):
                bi, d, j = units[i]
                pb, Tpb, pV, TpV, pden, r, n = ust.pop(i)
                ps2, Tps2 = P.ps8()
                for hh in range(2):
                    rows = slice(hh * 64, hh * 64 + 64)
                    p_own = pb[:, hh * 256:hh * 256 + 128]
                    p_prev = pb[:, hh * 256 + 128:hh * 256 + 256]
                    P.MM(ps2[rows, 0:128], pV[:, hh * 64:(hh + 1) * 64], p_prev, True, False, [TpV, Tpb], [Tps2])
                    P.MM(ps2[rows, 0:128], Vo[bi][:, j, hh * 64:(hh + 1) * 64], p_own, False, True, [TV[bi], Tpb], [Tps2])
                    P.MM(ps2[rows, 128:256], pden, p_prev, True, False, [self.Tc, Tpb], [Tps2])
                    P.MM(ps2[rows, 128:256], c["ones_b"][:, 0:64], p_own, False, True, [self.Tc, Tpb], [Tps2])
                start = n * 128 * d + r
                p_ap = ps2[:, 0:256].rearrange("p (a q) -> p a q", a=2)
                ev = "act" if i % 2 == 0 else "dve"
                if bi == 0:
                    P.CP(ev, accND[:, :, start:start + 128], p_ap, [Tps2], [Tacc])
                else:
                    P.CP(ev, tmpND[:, :, start:start + 127 * d + 1:d], p_ap, [Tps2], [Ttmp])
                    if j == 15:
                        P.TT("dve", accND.rearrange("p a t -> p (a t)"), accND.rearrange("p a t -> p (a t)"),
                             tmpND.rearrange("p a t -> p (a t)"), ALU.add, [Tacc, Ttmp], [Tacc])

            for i in range(len(units) + 2):
                if i < len(units):
                    stage_scores(i)
                if i >= 2:
                    stage_pv(i - 2)
            P.RECIP(accND[:, 1, :], accND[:, 1, :], [Tacc], [Tacc])
            P.TT("dve", mixT[:, hp, :], accND[:, 0, :], accND[:, 1, :], ALU.mult, [Tacc], [Tmix])

        if self.stop in ("attn", "b0"):
            Td = T("dbgmix")
            P.DMA("sp", self.dbg_mix, mixT.rearrange("p k t -> p (k t)"), [Tmix], [Td])
            self.final.append(Td)
            return
        P.transfer([Ttmp], [Tmix], c["scr"], self.Tscr)
        P.transfer(TPsb, Tstage, c["scr"], self.Tscr)
        old = TQ + TK + TKp + TV + TVp + [Tacc]
        o = R3
        memT = P.bf(o, 8 * 256).rearrange("p (k m) -> p k m", k=8); o += 1024
        kmT = P.bf(o, 2 * 256).rearrange("p (k m) -> p k m", k=2); o += 256
        vm = P.bf(o, 2 * 256).rearrange("p (k m) -> p k m", k=2); o += 256
        qmT = P.bf(o, 2 * TOK).rearrange("p (k t) -> p k t", k=2); o += TOK
        Pm = [[P.bf(o + (i * 2 + mc) * 256, 512) for mc in range(2)] for i in range(2)]; o += 1024
        rec = P.f32(o, 512); o += 512
        mb = P.f32(o, 8).rearrange("p (h m) -> p h m", h=4); o += 8
        TmemT, Tkm, Tvm, Tqm, Trec, Tmb = T("memT"), T("kmT"), T("vm"), T("qmT"), T("rec"), T("mb")
        TPm = [T("Pm0"), T("Pm1")]
        P.transfer(old, [TmemT, Tkm, Tvm, Tqm, Trec, Tmb] + TPm, c["scr"], self.Tscr)
        wkv = P.bf(R4, 8 * 512).rearrange("p (k n) -> p k n", k=8)
        wqm = P.bf(R4 + 2048, 8 * 256).rearrange("p (k n) -> p k n", k=8)
        Twkv, Twqm = T("wkv"), T("wqm")
        P.transfer(Twq, [Twkv, Twqm], c["scr"], self.Tscr)
        P.wload(wkv, self.w_mem_kv[l], [], [Twkv])
        P.wload(wqm, W_in[:, 2564:2820], [], [Twqm])
        P.DMA("sp", mb, self.mem_bias[l].rearrange("h (c m) -> m h c", c=2), [], [Tmb], slow=True)
        for mt in range(2):
            st, Tst = stage[mt], Tstage[mt]
            P.DMA("sp", st, self.mem_d[mt * 128:(mt + 1) * 128, :], [], [Tst])
            for half in range(2):
                ps, Tps = P.ps()
                for q in range(4):
                    kc = half * 4 + q
                    P.TR(ps[:, q * 128:(q + 1) * 128], st[:, kc * 128:(kc + 1) * 128], c["ident_f"], [Tst, self.Tc], [Tps])
                P.CP("act", memT[:, half * 4:(half + 1) * 4, mt * 128:(mt + 1) * 128], ps.rearrange("p (q t) -> p q t", q=4), [Tps], [TmemT])
        for ch in range(2):
            ps, Tps = P.ps()
            for kc in range(8):
                P.MM(ps[:, 0:256], wkv[:, kc, ch * 128:(ch + 1) * 128], memT[:, kc, :], kc == 0, kc == 7, [Twkv, TmemT], [Tps])
            P.CP("act", kmT[:, ch, :], ps[:, 0:256], [Tps], [Tkm])
        for mc in range(2):
            ps, Tps = P.ps()
            for kc in range(8):
                P.MM(ps[:, 0:256], memT[:, kc, mc * 128:(mc + 1) * 128], wkv[:, kc, 256:512], kc == 0, kc == 7, [Twkv, TmemT], [Tps])
            P.CP("dve", vm[:, mc, :], ps[:, 0:256], [Tps], [Tvm])
        for ch in range(2):
            for tg in range(4):
                ps, Tps = P.ps()
                for kc in range(8):
                    P.MM(ps, wqm[:, kc, ch * 128:(ch + 1) * 128], xT[:, kc, tg * 512:(tg + 1) * 512], kc == 0, kc == 7, [Twqm, TxT], [Tps])
                P.ACT(qmT[:, ch, tg * 512:(tg + 1) * 512], ps, AF.Copy, [Tps], [Tqm], scale=0.125)
        ui = 0
        for ch in range(2):
            for tg in range(4):
                psn, Tpsn = P.ps()
                psd, Tpsd = P.ps()
                for hh in range(2):
                    h = ch * 2 + hh
                    rows = slice(hh * 64, hh * 64 + 64)
                    pm, Tpm = Pm[ui % 2], TPm[ui % 2]
                    for mc in range(2):
                        ps, Tps = P.ps()
                        P.MM(ps, kmT[rows, ch, mc * 128:(mc + 1) * 128], qmT[rows, ch, tg * 512:(tg + 1) * 512], True, True, [Tkm, Tqm], [Tps])
                        P.ACT(pm[mc], ps, AF.Exp, [Tps, Tmb], [Tpm], bias=mb[:, h, mc:mc + 1])
                    for mc in range(2):
                        P.MM(psn[rows, :], vm[:, mc, h * 64:(h + 1) * 64], pm[mc], mc == 0, mc == 1, [Tvm, Tpm], [Tpsn])
                    for mc in range(2):
                        P.MM(psd[rows, :], c["ones_b"][:, 0:64], pm[mc], mc == 0, mc == 1, [self.Tc, Tpm], [Tpsd])
                    ui += 1
                P.RECIP(rec, psd, [Tpsd], [Trec])
                P.TT("dve", mixT[:, 6 + ch, tg * 512:(tg + 1) * 512], psn, rec, ALU.mult, [Tpsn, Trec], [Tmix])

        if self.stop == "mem":
            Td = T("dbgmix")
            P.DMA("sp", self.dbg_mix, mixT.rearrange("p k t -> p (k t)"), [Tmix], [Td])
            self.final.append(Td)
            return
        old = [TmemT, Tkm, Tvm, Tqm, Trec, Tmb] + TPm
        o = R3
        xbcT = P.bf(o, 6 * TOK).rearrange("p (k t) -> p k t", k=6); o += 3 * TOK
        XB = P.bf(o, 16 * 512).rearrange("p (j c) -> p j c", j=16); o += 16 * 256
        stg = [P.f32(o, 515), P.f32(o + 516, 515)]; o += 1032
        ctmp = P.f32(o, 512); o += 512
        cact = P.bf(o, 512); o += 256
        halo = P.f32(o, 18).rearrange("p (k c) -> p k c", k=6); o += 18
        cw = P.f32(o, 24).rearrange("p (k c) -> p k c", k=4); o += 24
        cb = P.f32(o, 6); o += 6
        dtb = P.f32(o, 4); o += 4
        Ab = P.f32(o, 4); o += 4
        dsk = P.f32(o, 4); o += 4
        nw = P.f32(o, 2); o += 2
        dt_sb = P.f32(o, 128); o += 128
        adt = P.f32(o, 128); o += 128
        cs = P.f32(o, 128); o += 128
        dec = P.f32(o, 128); o += 128
        ecs = P.f32(o, 128); o += 128
        dc = P.f32(o, 128); o += 128
        dtdec = P.f32(o, 128); o += 128
        Hst = P.f32(o, 256); o += 256
        Htmp = P.f32(o, 256); o += 256
        Hb = P.bf(o, 256); o += 128
        Xdt = P.bf(o, 256); o += 128
        Xdd = P.bf(o, 256); o += 128
        amask = P.f32(o, 512); o += 512
        expD = P.f32(o, 512); o += 512
        Msb = P.bf(o, 512); o += 256
        zs = P.f32(o, 256); o += 256
        y1 = P.f32(o, 256); o += 256
        y2 = P.f32(o, 256); o += 256
        ynb = P.bf(o, 256); o += 128
        ss = P.f32(o, 2); o += 2
        assert o <= R3 + 16384, o - R3
        names = "xbcT XB stg0 stg1 ctmp cact halo par dt adt cs dec ecs dc dtdec H Htmp Hb Xdt Xdd amask expD M zs y1 y2 ynb ss".split()
        tt_ = {n: T(n) for n in names}
        P.transfer(old, list(tt_.values()), c["scr"], self.Tscr)
        wx = [P.bf(R4 + i * 512, 8 * 128).rearrange("p (k n) -> p k n", k=8) for i in range(2)]
        Twx = [T("wx0"), T("wx1")]
        wz = P.bf(R4 + 1024, 8 * 256).rearrange("p (k n) -> p k n", k=8)
        wdt = P.bf(R4 + 2048, 8 * 4).rearrange("p (k n) -> p k n", k=8)
        Twz, Twdt = T("wz"), T("wdt")
        P.transfer([Twkv, Twqm], Twx + [Twz, Twdt], c["scr"], self.Tscr)
        Tpar = tt_["par"]
        P.wload(wz, W_in[:, 1536:1792], [], [Twz])
        P.wload(wdt, W_in[:, 2560:2564], [], [Twdt])
        P.DMA("sp", cw, self.conv_w[l].rearrange("k (c p) -> p k c", p=128), [], [Tpar], slow=True)
        P.DMA("sp", cb, self.conv_b[l:l + 1, :].rearrange("o (c p) -> p (o c)", p=128), [], [Tpar], slow=True)
        P.DMA("sp", dtb, self.dt_bias[l:l + 1, :].partition_broadcast(128), [], [Tpar])
        P.DMA("sp", Ab, self.a_log[l:l + 1, :].partition_broadcast(128), [], [Tpar])
        P.DMA("sp", dsk, self.d_skip[l:l + 1, :].partition_broadcast(128), [], [Tpar])
        P.DMA("sp", nw, self.ssd_nw[l:l + 1, :].rearrange("o (c p) -> p (o c)", p=128), [], [Tpar], slow=True)
        P.ACT(Ab, Ab, AF.Exp, [Tpar], [Tpar])
        P.TS("dve", Ab, Ab, -1.0, None, ALU.mult, None, [Tpar], [Tpar])
        pl, Tpl = self.plong
        for ti in range(32):
            src_xT, Tsrc = (xTp, TxTp) if ti < 16 else (xT, TxT)
            tl = ti % 16
            for kc in range(8):
                P.MM(pl[:, ti * 4:(ti + 1) * 4], src_xT[:, kc, tl * 128:(tl + 1) * 128], wdt[:, kc, :], kc == 0, kc == 7, [Twdt, Tsrc], [Tpl])
        v3 = lambda ap: ap.rearrange("p (t h) -> p t h", h=4)
        bc4 = lambda ap: ap.unsqueeze(1).to_broadcast([128, 32, 4])
        P.TT("dve", v3(dt_sb), v3(pl[:, 0:128]), bc4(dtb), ALU.add, [Tpl, Tpar], [tt_["dt"]])
        P.ACT(dt_sb, dt_sb, AF.Exp, [tt_["dt"]], [tt_["dt"]])
        P.ACT(dt_sb, dt_sb, AF.Ln, [tt_["dt"]], [tt_["dt"]], bias=1.0)
        P.TT("dve", v3(adt), v3(dt_sb), bc4(Ab), ALU.mult, [tt_["dt"], Tpar], [tt_["adt"]])
        P.MM(pl[:, 128:256], c["tri"], adt, True, True, [self.Tc, tt_["adt"]], [Tpl])
        P.MM(pl[:, 256:384], c["ones_f"], adt, True, True, [self.Tc, tt_["adt"]], [Tpl])
        P.CP("act", cs, pl[:, 128:256], [Tpl], [tt_["cs"]])
        P.TT("dve", dec, pl[:, 256:384], cs, ALU.subtract, [Tpl, tt_["cs"]], [tt_["dec"]])
        P.ACT(dec, dec, AF.Exp, [tt_["dec"]], [tt_["dec"]])
        P.ACT(ecs, cs, AF.Exp, [tt_["cs"]], [tt_["ecs"]])
        P.ACT(dc, pl[:, 256:384], AF.Exp, [Tpl], [tt_["dc"]])
        P.TT("dve", dtdec, dt_sb, dec, ALU.mult, [tt_["dt"], tt_["dec"]], [tt_["dtdec"]])
        P.MEMSET("pool", Hst, 0.0, [], [tt_["H"]])
        P.MEMSET("pool", halo, 0.0, [], [tt_["halo"]])

        bc64 = lambda ap: ap.unsqueeze(2).to_broadcast([128, 4, 64])
        v4 = lambda ap: ap.rearrange("p (h e) -> p h e", h=4)
        wxi = [0]
        ctmp2 = [ctmp, P.f32(R4 + 2064, 512)]
        Tctmp2 = [tt_["ctmp"], T("ctmp_b")]
        P.transfer([Twkv, Twqm], [Tctmp2[1]], c["scr"], self.Tscr)

        def conv_pass(cc, src_xT, Tsrc, tgs, own):
            wbuf, Twb_ = wx[wxi[0] % 2], Twx[wxi[0] % 2]
            wxi[0] += 1
            P.wload(wbuf, W_in[:, 1792 + cc * 128:1792 + (cc + 1) * 128], [], [Twb_])
            def proj(tg):
                ps, Tps = P.ps()
                for kc in range(8):
                    P.MM(ps, wbuf[:, kc, :], src_xT[:, kc, tg * 512:(tg + 1) * 512], kc == 0, kc == 7, [Twb_, Tsrc], [Tps])
                return ps, Tps

            def stage_a(gi, ps, Tps):
                sg, Tsg = stg[gi % 2], tt_["stg%d" % (gi % 2)]
                ct, Tct = ctmp2[gi % 2], Tctmp2[gi % 2]
                P.CP("dve", sg[:, 0:3], halo[:, cc, :], [tt_["halo"]], [Tsg])
                P.CP("act", sg[:, 3:515], ps, [Tps], [Tsg])
                P.CP("dve", halo[:, cc, :], sg[:, 512:515], [Tsg], [tt_["halo"]])
                P.ACT(ct, sg[:, 3:515], AF.Identity, [Tsg, Tpar], [Tct], scale=cw[:, 3, cc:cc + 1], bias=cb[:, cc:cc + 1])

            def stage_b(gi, tg):
                sg, Tsg = stg[gi % 2], tt_["stg%d" % (gi % 2)]
                ct, Tct = ctmp2[gi % 2], Tctmp2[gi % 2]
                for k in range(3):
                    P.STT("dve", ct, sg[:, k:k + 512], cw[:, k, cc:cc + 1], ct, ALU.mult, ALU.add, [Tsg, Tpar, Tct], [Tct])
                if own:
                    dst = xbcT[:, cc, tg * 512:(tg + 1) * 512]; Tdst = tt_["xbcT"]
                else:
                    dst = cact; Tdst = tt_["cact"]
                P.ACT(dst, ct, AF.Silu, [Tct], [Tdst])
                if cc < 4:
                    pb_, Tpb_ = self.pbf
                    for q in range(4):
                        P.TR(pb_[:, q * 128:(q + 1) * 128], dst[:, q * 128:(q + 1) * 128], c["ident_b"], [Tdst, self.Tc], [Tpb_])
                    P.CP("dve", XB[:, tg * 4:(tg + 1) * 4, cc * 128:(cc + 1) * 128], pb_[:, 0:512].rearrange("p (j c) -> p j c", j=4), [Tpb_], [tt_["XB"]])

            n_ = len(tgs)
            pj = {0: proj(tgs[0])}
            if n_ > 1:
                pj[1] = proj(tgs[1])
            stage_a(0, *pj.pop(0))
            for gi, tg in enumerate(tgs):
                if gi + 2 < n_:
                    pj[gi + 2] = proj(tgs[gi + 2])
                if gi + 1 < n_:
                    stage_a(gi + 1, *pj.pop(gi + 1))
                stage_b(gi, tg)

        o2 = R5
        zs2 = [zs, P.f32(o2, 256)]; o2 += 256
        amask2 = [amask, P.f32(o2, 512)]; o2 += 512
        expD2 = [expD, P.f32(o2, 512)]; o2 += 512
        Msb2 = [Msb, P.bf(o2, 512)]; o2 += 256
        Xdt2 = [Xdt, P.bf(o2, 256)]; o2 += 128
        Xdd2 = [Xdd, P.bf(o2, 256)]; o2 += 128
        assert o2 <= R5 + 2048
        names2 = "zs amask expD M Xdt Xdd".split()
        tt2 = {n: [tt_[n], T(n + "_b")] for n in names2}
        P.transfer(Tstage, [tt2[n][1] for n in names2], c["scr"], self.Tscr)

        def state_front(ti, par):
            tl = ti % 16
            col = slice(ti * 4, ti * 4 + 4)
            xdd, Txdd = Xdd2[par], tt2["Xdd"][par]
            P.TT("dve", v4(xdd), v4(XB[:, tl, 0:256]), bc64(dtdec[:, col]), ALU.mult, [tt_["XB"], tt_["dtdec"]], [Txdd])
            ps, Tps = P.ps()
            for g in range(2):
                P.MM(ps[:, g * 128:(g + 1) * 128], XB[:, tl, 256 + g * 128:256 + (g + 1) * 128], xdd[:, g * 128:(g + 1) * 128], True, True,
                     [tt_["XB"], Txdd], [Tps])
            return ps, Tps

        def state_back(ti, ps, Tps):
            col = slice(ti * 4, ti * 4 + 4)
            P.TT("dve", v4(Htmp), v4(Hst), bc64(dc[:, col]), ALU.mult, [tt_["H"], tt_["dc"]], [tt_["Htmp"]])
            P.TT("dve", Hst, Htmp, ps[:, 0:256], ALU.add, [tt_["Htmp"], Tps], [tt_["H"]])

        for cc in range(4):
            conv_pass(cc, xTp, TxTp, [0, 1, 2, 3], False)
        for cc in (4, 5):
            conv_pass(cc, xTp, TxTp, [3], False)
        pend = state_front(0, 0)
        for ti in range(16):
            nxt = state_front(ti + 1, (ti + 1) % 2) if ti + 1 < 16 else None
            state_back(ti, *pend)
            pend = nxt
        P.TS("dve", Hst, Hst, c["flag"][:, 0:1], None, ALU.mult, None, [tt_["H"], self.Tflag], [tt_["H"]])
        P.TS("dve", halo.rearrange("p k c -> p (k c)"), halo.rearrange("p k c -> p (k c)"), c["flag"][:, 0:1], None, ALU.mult, None,
             [tt_["halo"], self.Tflag], [tt_["halo"]])
        for cc in range(6):
            conv_pass(cc, xT, TxT, [0, 1, 2, 3], True)

        def own_front1(tl, par):
            ti = 16 + tl
            col = slice(ti * 4, ti * 4 + 4)
            tok = slice(tl * 128, (tl + 1) * 128)
            ps1, Tps1 = P.ps()
            for kc in range(8):
                P.MM(ps1[:, 0:256], xT[:, kc, tok], wz[:, kc, :], kc == 0, kc == 7, [Twz, TxT], [Tps1])
            for g in range(2):
                P.MM(ps1[:, 256 + g * 128:256 + (g + 1) * 128], xbcT[:, 2 + g, tok], xbcT[:, 4 + g, tok], True, True, [tt_["xbcT"]], [Tps1])
            P.ACT(zs2[par], ps1[:, 0:256], AF.Exp, [Tps1], [tt2["zs"][par]], scale=-1.0)
            am, Tam = amask2[par], tt2["amask"][par]
            psD, TpsD = P.ps()
            for h in range(4):
                P.TS("dve", am[:, h * 128:(h + 1) * 128], c["su"], adt[:, ti * 4 + h:ti * 4 + h + 1], None, ALU.mult, None,
                     [self.Tc, tt_["adt"]], [Tam])
            for h in range(4):
                P.MM(psD[:, h * 128:(h + 1) * 128], am[:, h * 128:(h + 1) * 128], c["tri"], True, False, [Tam, self.Tc], [TpsD])
                P.MM(psD[:, h * 128:(h + 1) * 128], c["ident_f"], c["neg"], False, True, [self.Tc], [TpsD])
            P.ACT(expD2[par], psD, AF.Exp, [TpsD], [tt2["expD"][par]])
            return ps1, Tps1

        def own_front2(tl, par, f1):
            ti = 16 + tl
            col = slice(ti * 4, ti * 4 + 4)
            ps1, Tps1 = f1
            P.TS("dve", zs2[par], zs2[par], 1.0, None, ALU.add, None, [tt2["zs"][par]], [tt2["zs"][par]])
            P.RECIP(zs2[par], zs2[par], [tt2["zs"][par]], [tt2["zs"][par]])
            P.TT("dve", zs2[par], ps1[:, 0:256], zs2[par], ALU.mult, [Tps1, tt2["zs"][par]], [tt2["zs"][par]])
            P.TT("dve", Msb2[par].rearrange("p (g h l) -> p g h l", g=2, h=2), expD2[par].rearrange("p (g h l) -> p g h l", g=2, h=2),
                 ps1[:, 256:512].rearrange("p (g l) -> p g l", g=2).unsqueeze(2).to_broadcast([128, 2, 2, 128]), ALU.mult,
                 [tt2["expD"][par], Tps1], [tt2["M"][par]])
            P.TT("dve", v4(Xdt2[par]), v4(XB[:, tl, 0:256]), bc64(dt_sb[:, col]), ALU.mult, [tt_["XB"], tt_["dt"]], [tt2["Xdt"][par]])
            return state_front(ti, par) if tl < 15 else None

        def own_back(tl, par, st):
            ti = 16 + tl
            col = slice(ti * 4, ti * 4 + 4)
            tok = slice(tl * 128, (tl + 1) * 128)
            msb, Tmsb = Msb2[par], tt2["M"][par]
            xdt, Txdt = Xdt2[par], tt2["Xdt"][par]
            P.CP("pool", Hb, Hst, [tt_["H"]], [tt_["Hb"]])
            psY, TpsY = P.ps()
            for h in range(4):
                P.MM(psY[:, h * 64:(h + 1) * 64], msb[:, h * 128:(h + 1) * 128], xdt[:, h * 64:(h + 1) * 64], True, True, [Tmsb, Txdt], [TpsY])
            for g in range(2):
                P.MM(psY[:, 256 + g * 128:256 + (g + 1) * 128], xbcT[:, 4 + g, tok], Hb[:, g * 128:(g + 1) * 128], True, True, [tt_["xbcT"], tt_["Hb"]], [TpsY])
            P.TT("dve", v4(y1), v4(psY[:, 256:512]), bc64(ecs[:, col]), ALU.mult, [TpsY, tt_["ecs"]], [tt_["y1"]])
            P.TT("dve", y1, y1, psY[:, 0:256], ALU.add, [tt_["y1"], TpsY], [tt_["y1"]])
            P.TT("dve", v4(y2), v4(XB[:, tl, 0:256]), bc64(dsk), ALU.mult, [tt_["XB"], Tpar], [tt_["y2"]])
            P.TT("dve", y1, y1, y2, ALU.add, [tt_["y1"], tt_["y2"]], [tt_["y1"]])
            P.TT("dve", y1, y1, zs2[par], ALU.mult, [tt_["y1"], tt2["zs"][par]], [tt_["y1"]])
            P.MEMSET("dve", ss[:, 0:1], 0.0, [], [tt_["ss"]])
            P.ACT(y2, y1, AF.Square, [tt_["y1"], tt_["y2"], tt_["ss"]], [tt_["y2"], tt_["ss"]], accum_out=ss[:, 0:1], scale=1.0 / 16.0)
            P.ACT(ss[:, 1:2], ss[:, 0:1], AF.Ln, [tt_["ss"]], [tt_["ss"]], bias=RMS_EPS)
            P.ACT(ss[:, 1:2], ss[:, 1:2], AF.Exp, [tt_["ss"]], [tt_["ss"]], scale=-0.5)
            P.TS("dve", ynb, y1, ss[:, 1:2], None, ALU.mult, None, [tt_["y1"], tt_["ss"]], [tt_["ynb"]])
            pb_, Tpb_ = self.pbf
            for q in range(2):
                P.TR(pb_[:, q * 128:(q + 1) * 128], ynb[:, q * 128:(q + 1) * 128], c["ident_b"], [tt_["ynb"], self.Tc], [Tpb_])
            for q in range(2):
                P.ACT(mixT[:, 4 + q, tok], pb_[:, q * 128:(q + 1) * 128], AF.Identity, [Tpb_, Tpar], [Tmix], scale=nw[:, q:q + 1])
            if st is not None:
                state_back(ti, *st)

        pend = own_front2(0, 0, own_front1(0, 0))
        for tl in range(16):
            f1 = own_front1(tl + 1, (tl + 1) % 2) if tl + 1 < 16 else None
            own_back(tl, tl % 2, pend)
            pend = own_front2(tl + 1, (tl + 1) % 2, f1) if tl + 1 < 16 else None
        P.transfer([tt2[n][1] for n in names2], Tstage, c["scr"], self.Tscr)

        if self.debug:
            Td = T("dbgmix")
            P.DMA("sp", self.dbg_mix, mixT.rearrange("p k t -> p (k t)"), [Tmix], [Td])
            self.final.append(Td)

        if self.stop == "ssd":
            return
        old = list(tt_.values())
        acc = P.f32(R3, 16 * 1024).rearrange("p (j c) -> p j c", j=16)
        Tacc2 = [T("acc%d" % j) for j in range(16)]
        P.transfer(old, Tacc2, c["scr"], self.Tscr)
        wout = P.bf(R4, 8 * 1024).rearrange("p (k n) -> p k n", k=8)
        Twout = T("wout")
        P.transfer(Twx + [Twz, Twdt, Tctmp2[1]], [Twout], c["scr"], self.Tscr)
        P.wload(wout, self.w_out[l], [], [Twout])
        o = R1
        lng = P.f32(o, 1024); o += 1024
        lnb = P.f32(o, 1024); o += 1024
        lo = P.bf(o, 8 * 128).rearrange("p (k t) -> p k t", k=8); o += 512
        wr_f = P.f32(o, 128).rearrange("p (k n) -> p k n", k=8); o += 128
        wr_hi = P.bf(o, 128).rearrange("p (k n) -> p k n", k=8); o += 64
        wr_lo = P.bf(o, 128).rearrange("p (k n) -> p k n", k=8); o += 64
        rsb = [P.f32(o, 1024), P.f32(o + 1024, 1024)]; o += 2048
        bst = P.f32(o, 12); o += 12
        mv = P.f32(o, 4); o += 4
        Tln, Tlo, Twr = T("ln1"), T("lo"), T("wr")
        Trsb = [T("rsb0"), T("rsb1")]
        Tbst = T("bst")
        P.transfer([TxTp], [Tln, Tlo, Twr, Tbst] + Trsb, c["scr"], self.Tscr)
        P.DMA("sp", lng, self.ln1_g[l:l + 1, :].partition_broadcast(128), [], [Tln])
        P.DMA("sp", lnb, self.ln1_b[l:l + 1, :].partition_broadcast(128), [], [Tln])
        P.DMA("sp", wr_f, self.w_router.rearrange("(k p) n -> p k n", p=128), [], [Twr])
        P.CP("dve", wr_hi, wr_f, [Twr], [Twr])
        P.TT("dve", wr_f, wr_f, wr_hi, ALU.subtract, [Twr], [Twr])
        P.CP("dve", wr_lo, wr_f, [Twr], [Twr])
        gates = P.f32(small, 256).rearrange("p (t e) -> p t e", t=16)
        gtmp = [P.f32(small + 256 + i * 64, 64) for i in range(4)]
        Tg = T("gates")
        x1T = xT
        Tx1T = [T("x1T%d" % g) for g in range(4)]
        P.transfer([TxT], Tx1T, c["scr"], self.Tscr)
        pl, Tpl = self.plong

        def layer_norm(r, Tr, g_ap, b_ap, Tgb, out_ap, Tout_list, bst, mv, Tbst):
            P.BNS(bst[:, 0:6], r[:, 0:512], [Tr], [Tbst])
            P.BNS(bst[:, 6:12], r[:, 512:1024], [Tr], [Tbst])
            P.BNA(mv[:, 0:2], bst.rearrange("p (a b) -> p a b", a=2), [Tbst], [Tbst])
            P.ACT(mv[:, 2:3], mv[:, 1:2], AF.Sqrt, [Tbst], [Tbst], bias=LN_EPS)
            P.RECIP(mv[:, 2:3], mv[:, 2:3], [Tbst], [Tbst])
            P.STT("dve", r, r, mv[:, 0:1], g_ap, ALU.subtract, ALU.mult, [Tr, Tbst, Tgb], [Tr])
            P.STT("dve", out_ap, r, mv[:, 2:3], b_ap, ALU.mult, ALU.add, [Tr, Tbst, Tgb], Tout_list)

        wps = {}

        def wout_mm(tt):
            tok = slice(tt * 128, (tt + 1) * 128)
            st, Tst = stage[tt % 2], Tstage[tt % 2]
            x_ap, x_R = x_own(tt)
            P.DMA("sp", st, x_ap, x_R, [Tst])
            wps[tt] = []
            for half in range(2):
                ps, Tps = P.ps()
                for kc in range(8):
                    P.MM(ps, mixT[:, kc, tok], wout[:, kc, half * 512:(half + 1) * 512], kc == 0, kc == 7, [Tmix, Twout], [Tps])
                wps[tt].append((ps, Tps))

        def wout_res(tt):
            st, Tst = stage[tt % 2], Tstage[tt % 2]
            r, Tr = rsb[tt % 2], Trsb[tt % 2]
            for half, (ps, Tps) in enumerate(wps.pop(tt)):
                P.STT("dve", r[:, half * 512:(half + 1) * 512], st[:, half * 512:(half + 1) * 512], ALPHA, ps, ALU.mult, ALU.add, [Tst, Tps], [Tr])

        wout_mm(0)
        wout_res(0)
        for tt in range(NT):
            tok = slice(tt * 128, (tt + 1) * 128)
            r, Tr = rsb[tt % 2], Trsb[tt % 2]
            if tt + 1 < NT:
                wout_mm(tt + 1)
            layer_norm(r, Tr, lng, lnb, Tln, r, [Tr], bst, mv, Tbst)
            if tt + 1 < NT:
                wout_res(tt + 1)
            if self.debug:
                Td = T("dbgx1_%d" % tt)
                P.DMA("sp", self.dbg_x1[tok, :], r, [Tr], [Td])
                self.final.append(Td)
            P.ACT(acc[:, tt, :], r, AF.Copy, [Tr], [Tacc2[tt]], scale=ALPHA)
            for half in range(2):
                ps, Tps = P.ps()
                for q in range(4):
                    kc = half * 4 + q
                    P.TR(ps[:, q * 128:(q + 1) * 128], r[:, kc * 128:(kc + 1) * 128], c["ident_f"], [Tr, self.Tc], [Tps])
                hi_ap = x1T[:, half * 4:(half + 1) * 4, tok]
                psv = ps.rearrange("p (q t) -> p q t", q=4)
                P.CP("act", hi_ap, psv, [Tps], [Tx1T[tt // 4]])
                P.TT("dve", lo[:, half * 4:(half + 1) * 4, :], psv, hi_ap, ALU.subtract, [Tps, Tx1T[tt // 4]], [Tlo])
            k = 0
            for (a_, w_) in ((0, wr_hi), (1, wr_hi), (0, wr_lo)):
                for kc in range(8):
                    lhs = x1T[:, kc, tok] if a_ == 0 else lo[:, kc, :]
                    P.MM(pl[:, tt * 16:(tt + 1) * 16], lhs, w_[:, kc, :], k == 0, k == 23, [Tx1T[tt // 4], Tlo, Twr], [Tpl])
                    k += 1
        brt = P.f32(small + 512, 16)
        Tbrt = T("brt")
        P.DMA("sp", brt, self.b_router.partition_broadcast(128), [], [Tbrt])
        g3 = gates
        sc = P.f32(small + 528, 0) if False else None
        lg = P.f32(R1 + 6000, 256).rearrange("p (t e) -> p t e", t=16)
        t1 = P.f32(R1 + 6256, 256).rearrange("p (t e) -> p t e", t=16)
        t2 = P.f32(R1 + 6512, 256).rearrange("p (t e) -> p t e", t=16)
        m1 = P.f32(R1 + 6768, 64).rearrange("p (t g) -> p t g", t=16)
        m2 = P.f32(R1 + 6832, 64).rearrange("p (t g) -> p t g", t=16)
        gs = P.f32(R1 + 6896, 64).rearrange("p (t g) -> p t g", t=16)
        mx = P.f32(R1 + 6960, 16)
        Tgt = T("gtmp")
        P.transfer([], [Tgt], c["scr"], self.Tscr)
        P.TT("dve", lg, pl[:, 0:256].rearrange("p (t e) -> p t e", t=16), brt.unsqueeze(1).to_broadcast([128, 16, 16]), ALU.add, [Tpl, Tbrt], [Tgt])
        P.REDUCE(mx, lg, ALU.max, [Tgt], [Tgt])
        P.TT("dve", lg, lg, mx.unsqueeze(2).to_broadcast([128, 16, 16]), ALU.subtract, [Tgt], [Tgt])
        P.ACT(lg, lg, AF.Exp, [Tgt], [Tgt])
        P.REDUCE(mx, lg, ALU.add, [Tgt], [Tgt])
        P.RECIP(mx, mx, [Tgt], [Tgt])
        P.TT("dve", lg, lg, mx.unsqueeze(2).to_broadcast([128, 16, 16]), ALU.mult, [Tgt], [Tgt])
        lg4 = lg.rearrange("p t (g k) -> p t g k", g=4)
        t14 = t1.rearrange("p t (g k) -> p t g k", g=4)
        t24 = t2.rearrange("p t (g k) -> p t g k", g=4)
        bcg = lambda ap: ap.unsqueeze(3).to_broadcast([128, 16, 4, 4])
        P.REDUCE(m1, lg4, ALU.max, [Tgt], [Tgt])
        P.TT("dve", t14, lg4, bcg(m1), ALU.is_ge, [Tgt], [Tgt])
        P.STT("dve", t14, t14, -4.0, lg4, ALU.mult, ALU.add, [Tgt], [Tgt])
        P.REDUCE(m2, t14, ALU.max, [Tgt], [Tgt])
        P.TT("dve", gs, m1, m2, ALU.add, [Tgt], [Tgt])
        P.REDUCE(mx, gs, ALU.max, [Tgt], [Tgt])
        P.TT("dve", m1, gs, mx.unsqueeze(2).to_broadcast([128, 16, 4]), ALU.is_ge, [Tgt], [Tgt])
        P.TT("dve", t24, lg4, bcg(m2), ALU.is_ge, [Tgt], [Tgt])
        P.TT("dve", t24, t24, bcg(m1), ALU.mult, [Tgt], [Tgt])
        P.TT("dve", t2, t2, lg, ALU.mult, [Tgt], [Tgt])
        P.RECIP(mx, mx, [Tgt], [Tgt])
        P.TT("dve", g3, t2, mx.unsqueeze(2).to_broadcast([128, 16, 16]), ALU.mult, [Tgt], [Tg])

        if self.stop == "gate":
            Td = T("dbgg")
            P.DMA("sp", self.out_d[0:128, 0:256], gates.rearrange("p t e -> p (t e)"), [Tg], [Td])
            self.final.append(Td)
            return
        gh = mixT
        Tgh = [T("gh%d" % g) for g in range(4)]
        P.transfer([Tmix], Tgh, c["scr"], self.Tscr)
        slots = [P.bf(R1, 8 * 1024).rearrange("p (k n) -> p k n", k=8),
                 P.bf(R1 + 4096, 8 * 1024).rearrange("p (k n) -> p k n", k=8),
                 P.bf(R4, 8 * 1024).rearrange("p (k n) -> p k n", k=8)]
        Tslot = [T("slot0"), T("slot1"), T("slot2")]
        P.transfer([Tln, Tlo, Twr, Tbst, Tgt] + Trsb, Tslot[0:2], c["scr"], self.Tscr)
        P.transfer([Twout], Tslot[2:3], c["scr"], self.Tscr)
        mi = 0
        for e_ in range(NE):
            mats = (self.w_gate[l, e_], self.w_up[l, e_], self.w_down[l, e_])
            sl = []
            for m_ in range(3):
                s_i = mi % 3
                mi += 1
                P.wload(slots[s_i], mats[m_], [], [Tslot[s_i]])
                sl.append((slots[s_i], Tslot[s_i]))
            (wg_, Twg_), (wu_, Twu_), (wd_, Twd_) = sl
            for tg in range(4):
                tk = slice(tg * 512, (tg + 1) * 512)
                for fc in range(8):
                    ps, Tps = P.ps()
                    for kc in range(8):
                        P.MM(ps, wg_[:, kc, fc * 128:(fc + 1) * 128], x1T[:, kc, tk], kc == 0, kc == 7, [Twg_, Tx1T[tg]], [Tps])
                    P.ACT(gh[:, fc, tk], ps, AF.Silu, [Tps], [Tgh[tg]])
            for tg in range(4):
                tk = slice(tg * 512, (tg + 1) * 512)
                for fc in range(8):
                    ps, Tps = P.ps()
                    for kc in range(8):
                        P.MM(ps, wu_[:, kc, fc * 128:(fc + 1) * 128], x1T[:, kc, tk], kc == 0, kc == 7, [Twu_, Tx1T[tg]], [Tps])
                    P.TT("dve", gh[:, fc, tk], ps, gh[:, fc, tk], ALU.mult, [Tps, Tgh[tg]], [Tgh[tg]])
            if e_ == NE - 1:
                lng2 = P.f32(R0, 1024)
                lnb2 = P.f32(R0 + 1024, 1024)
                Tln2 = T("ln2")
                P.transfer(Tx1T, [Tln2], c["scr"], self.Tscr)
                P.DMA("sp", lng2, self.ln2_g[l:l + 1, :].partition_broadcast(128), [], [Tln2])
                P.DMA("sp", lnb2, self.ln2_b[l:l + 1, :].partition_broadcast(128), [], [Tln2])
                ost = stage
                Tost = Tstage
                bst2 = P.f32(small + 532, 12)
                mv2 = P.f32(small + 548, 4)
                Tbst2 = T("bst2")
            for tt in range(NT):
                tok = slice(tt * 128, (tt + 1) * 128)
                for half in range(2):
                    ps, Tps = P.ps()
                    for fc in range(8):
                        P.MM(ps, gh[:, fc, tok], wd_[:, fc, half * 512:(half + 1) * 512], fc == 0, fc == 7, [Tgh[tt // 4], Twd_], [Tps])
                    a_ap = acc[:, tt, half * 512:(half + 1) * 512]
                    P.STT("dve", a_ap, ps, gates[:, tt, e_:e_ + 1], a_ap, ALU.mult, ALU.add, [Tps, Tg, Tacc2[tt]], [Tacc2[tt]])
                if e_ == NE - 1:
                    r = acc[:, tt, :]
                    layer_norm(r, Tacc2[tt], lng2, lnb2, Tln2, ost[tt % 2], [Tost[tt % 2]], bst2, mv2, Tbst2)
                    o_ap, To = out_d(tt)
                    P.DMA("sp", o_ap, ost[tt % 2], [Tost[tt % 2]], [To])
        self.apos = base

    def barrier(self):
        P = self
        c = self.c
        old = list(_ALLT)
        Tb = T("barrier")
        bar = c["bar"]
        self.S.op("pool", lambda e: e.memset(bar[:, 0:1], 0.0), [], [Tb] + old)
        P.ACT(bar[:, 1:2], bar[:, 0:1], AF.Copy, [Tb], [T("bar_act")])
        P.CP("dve", bar[:, 2:3], bar[:, 0:1], [Tb], [T("bar_dve")])
        P.DMA("sp", bar[:, 3:4], self.flag_d, [Tb], [T("bar_sp")])
        for i, (ps, Tps) in enumerate(self.pbanks):
            P.MM(ps[:, 0:1], c["ones_f"], bar[:, 0:1], True, True, [Tb, self.Tc], [Tps])
            P.CP("act", bar[:, 4 + i:5 + i], ps[:, 0:1], [Tps], [T("bar_ps%d" % i)])

    def build(self, layer_ids):
        self.final = []
        self.setup()
        nl = len(layer_ids)
        Tout = T("out")
        x_own = lambda tt: (self.x_own[tt * 128:(tt + 1) * 128, :], [])
        x_pre = lambda tt: (self.x_pre[tt * 128:(tt + 1) * 128, :], [])
        for i, l in enumerate(layer_ids):
            last = i == nl - 1
            if last:
                out_fn = lambda tt: (self.out_d[tt * 128:(tt + 1) * 128, :], Tout)
            else:
                Txm = [T("xmid%d_%d" % (i, cch)) for cch in range(4)]
                out_fn = (lambda Txm: (lambda tt: (self.xmid[tt * 128:(tt + 1) * 128, :], Txm[tt // 4])))(Txm)
            self.layer(l, x_own, x_pre, out_fn)
            if not last:
                Txg = [T("xg%d_%d" % (i, cch)) for cch in range(4)]
                for cch in range(4):
                    src = self.xmid[cch * 512:(cch + 1) * 512, :]
                    dst = self.xg[cch * 1024:(cch + 1) * 1024, :]
                    self.S.op("pool", (lambda src, dst: (lambda e: e.collective_compute(
                        "AllGather", ALU.bypass, replica_groups=[[0, 1], [2, 3], [4, 5], [6, 7]], ins=[src], outs=[dst])))(src, dst),
                        [Txm[cch]], [Txg[cch]], dma=True, inc=1)
                self.barrier()
                x_own = (lambda Txm: (lambda tt: (self.xmid[tt * 128:(tt + 1) * 128, :], [Txm[tt // 4]])))(Txm)
                x_pre = (lambda Txg: (lambda tt: (self.xg[(tt // 4) * 1024 + (tt % 4) * 128:(tt // 4) * 1024 + (tt % 4) * 128 + 128, :],
                                                  [Txg[tt // 4]])))(Txg)
        self.final.append(Tout)
        self.S.emit(final_tiles=self.final)
        return self.nc


_CACHE = {}


def _get_prog():
    if "fused" not in _CACHE:
        p = Prog(DEPTH)
        p.build(list(range(DEPTH)))
        _CACHE["fused"] = p
    return _CACHE["fused"]


def _core_inputs(inputs, oh):
    f = lambda a: np.ascontiguousarray(np.asarray(a, dtype=np.float32))
    x = f(inputs["x"])
    shared = dict(
        w_in=f(inputs["w_in"]), w_out=f(inputs["w_out"]), rel_bias=f(inputs["rel_bias"]), oh=oh,
        conv_w=f(inputs["conv_w"]), conv_b=f(inputs["conv_b"]), dt_bias=f(inputs["dt_bias"]),
        a_log=f(inputs["a_log"]), d_skip=f(inputs["d_skip"]), ssd_norm_w=f(inputs["ssd_norm_w"]),
        w_mem_kv=f(inputs["w_mem_kv"]), mem_bias=f(inputs["mem_bias"]),
        ln1_g=f(inputs["ln1_g"]), ln1_b=f(inputs["ln1_b"]), ln2_g=f(inputs["ln2_g"]), ln2_b=f(inputs["ln2_b"]),
        w_router=f(inputs["w_router"]), b_router=f(inputs["b_router"]).reshape(1, NE),
        w_gate=f(inputs["w_gate"]), w_up=f(inputs["w_up"]), w_down=f(inputs["w_down"]),
    )
    zeros = np.zeros((TOK, D), np.float32)
    maps = []
    for core in range(8):
        b, h = core // 2, core % 2
        m = dict(shared)
        m["x_own"] = np.ascontiguousarray(x[b, h * TOK:(h + 1) * TOK])
        m["x_pre"] = np.ascontiguousarray(x[b, 0:TOK]) if h == 1 else zeros
        m["flag"] = np.full((128, 1), float(h), np.float32)
        m["mem"] = f(inputs["mem"][b])
        maps.append(m)
    return maps


def kernel(**inputs):
    oh = _onehot_table()
    prog = _get_prog()
    maps = _core_inputs(inputs, oh)
    res = run_bass_kernel_spmd(prog.nc, maps, core_ids=list(range(8)))
    out = np.empty((4, 2 * TOK, D), np.float32)
    for core in range(8):
        b, h = core // 2, core % 2
        out[b, h * TOK:(h + 1) * TOK] = res.results[core]["x_out"]
    return out
```
